# Optimizing a Trainium2 kernel written in Bass

```python
import math
import jax, jax.numpy as jnp
from jax import lax
import numpy as np

D_MODEL = 2048
BATCH = 2
SEQ = 4096
DEPTH = 1

N_META = 16
BLOCK = 128
EPS = 1e-6
ATT_HEADS = 8
ATT_SUB_DIM = 64
ATT_V_DIM = 2 * ATT_SUB_DIM
ATT_WIDTH = ATT_HEADS * 2 * ATT_SUB_DIM
ROPE_THETA = 10000.0
SSM_WIDTH = D_MODEL // 2
SSM_GROUP = 16
SSM_GROUPS = SSM_WIDTH // SSM_GROUP
SSM_STATE = 64
PEER_HEADS = 8
PEER_KEYS = 128
PEER_EXPERTS = PEER_KEYS * PEER_KEYS
PEER_QDIM = 256
PEER_HALF = PEER_QDIM // 2
PEER_TOPK = 16
IN_WIDTH = 3 * ATT_WIDTH + SSM_WIDTH + 2 * D_MODEL
IN_SPLITS = [ATT_WIDTH, 2 * ATT_WIDTH, 3 * ATT_WIDTH, 3 * ATT_WIDTH + SSM_WIDTH, 3 * ATT_WIDTH + SSM_WIDTH + D_MODEL]

kernel_name = "hybrid_diffattn_s5_peer_block"


def rms_norm(x, g):
    xf = x.astype(jnp.float32)
    y = xf * lax.rsqrt(jnp.mean(xf * xf, axis=-1, keepdims=True) + EPS)
    return (y * g.astype(jnp.float32)).astype(x.dtype)


def rotary(t, pos):
    half = t.shape[-1] // 2
    inv_freq = jnp.power(ROPE_THETA, -jnp.arange(half, dtype=jnp.float32) / half)
    ang = pos.astype(jnp.float32)[:, None] * inv_freq[None, :]
    cos = jnp.cos(ang)[:, None, None, :]
    sin = jnp.sin(ang)[:, None, None, :]
    tf = t.astype(jnp.float32)
    t1, t2 = tf[..., :half], tf[..., half:]
    return jnp.concatenate([t1 * cos - t2 * sin, t2 * cos + t1 * sin], axis=-1).astype(t.dtype)


def diff_attention(q, k, v, lam):
    B, Lp = q.shape[0], q.shape[1]
    nb = Lp // BLOCK
    scale = ATT_SUB_DIM ** -0.5
    qb = q.reshape(B, nb, BLOCK, ATT_HEADS, 2, ATT_SUB_DIM).transpose(1, 0, 2, 3, 4, 5)
    kpos = jnp.arange(Lp)

    def one_block(args):
        qi, i = args
        s = jnp.einsum('bqhcd,bkhcd->bhcqk', qi, k, preferred_element_type=jnp.float32) * scale
        qpos = i * BLOCK + jnp.arange(BLOCK)
        mask = kpos[None, :] <= qpos[:, None]
        s = jnp.where(mask, s, -1e30)
        p = jax.nn.softmax(s, axis=-1)
        w = p[:, :, 0] - lam * p[:, :, 1]
        return jnp.einsum('bhqk,bkhd->bqhd', w.astype(v.dtype), v)

    out = lax.map(one_block, (qb, jnp.arange(nb)))
    return out.transpose(1, 0, 2, 3, 4).reshape(B, Lp, ATT_HEADS, ATT_V_DIM)


def s5_ssm(u, a_re, a_im, log_dt, b_re, b_im, c_re, c_im, d_skip):
    B, L = u.shape[0], u.shape[1]
    uf = u.astype(jnp.float32).reshape(B, L, SSM_GROUPS, SSM_GROUP)
    lam = lax.complex(a_re.astype(jnp.float32), a_im.astype(jnp.float32))
    dt = jnp.exp(log_dt.astype(jnp.float32))[:, None]
    a_bar = jnp.exp(lam * dt)
    b = lax.complex(b_re.astype(jnp.float32), b_im.astype(jnp.float32))
    b_bar = ((a_bar - 1.0) / lam)[..., None] * b
    bu = jnp.einsum('gpc,blgc->blgp', b_bar, uf.astype(jnp.complex64))
    a_elems = jnp.broadcast_to(a_bar, bu.shape)

    def combine(e1, e2):
        a1, s1 = e1
        a2, s2 = e2
        return a1 * a2, a2 * s1 + s2

    _, states = lax.associative_scan(combine, (a_elems, bu), axis=1)
    c = lax.complex(c_re.astype(jnp.float32), c_im.astype(jnp.float32))
    y = jnp.einsum('gcp,blgp->blgc', c, states).real.reshape(B, L, SSM_WIDTH)
    y = y + d_skip.astype(jnp.float32) * u.astype(jnp.float32)
    return y.astype(u.dtype)


def peer(h, w_q, sub_k1, sub_k2, u_emb, v_emb):
    B, L, D = h.shape
    q = (h @ w_q).reshape(B, L, PEER_HEADS, 2, PEER_HALF)
    s1 = jnp.einsum('blhd,hnd->blhn', q[..., 0, :], sub_k1, preferred_element_type=jnp.float32)
    s2 = jnp.einsum('blhd,hnd->blhn', q[..., 1, :], sub_k2, preferred_element_type=jnp.float32)
    v1, i1 = lax.top_k(s1, PEER_TOPK)
    v2, i2 = lax.top_k(s2, PEER_TOPK)
    cand = (v1[..., :, None] + v2[..., None, :]).reshape(B, L, PEER_HEADS, PEER_TOPK * PEER_TOPK)
    cand_idx = (i1[..., :, None] * PEER_KEYS + i2[..., None, :]).reshape(B, L, PEER_HEADS, PEER_TOPK * PEER_TOPK)
    vals, sel = lax.top_k(cand, PEER_TOPK)
    idx = jnp.take_along_axis(cand_idx, sel, axis=-1)
    g = jax.nn.softmax(vals, axis=-1)
    T = B * L
    nb = T // BLOCK
    hb = h.reshape(nb, BLOCK, D)
    ib = idx.reshape(nb, BLOCK, PEER_HEADS * PEER_TOPK)
    gb = g.reshape(nb, BLOCK, PEER_HEADS * PEER_TOPK).astype(h.dtype)

    def one_block(args):
        hi, ii, gi = args
        u = u_emb[ii]
        a = jnp.einsum('td,tkd->tk', hi, u)
        w = gi * jax.nn.gelu(a, approximate=False)
        return jnp.einsum('tk,tkd->td', w, v_emb[ii])

    out = lax.map(one_block, (hb, ib, gb))
    return out.reshape(B, L, D)


def setup_inputs(seed: int = 0) -> dict:
    key = jax.random.key(seed)
    ks = jax.random.split(key, 32)
    f32 = jnp.float32
    nrm = lambda k, shape, s: jax.random.normal(k, shape, f32) * s
    gain = lambda k, shape: 1.0 + 0.01 * jax.random.normal(k, shape, f32)
    Dp = DEPTH
    a_im_base = jnp.pi * jnp.arange(SSM_STATE, dtype=f32)
    return {
        "x": nrm(ks[0], (BATCH, SEQ, D_MODEL), 1.0),
        "meta_tokens": nrm(ks[1], (N_META, D_MODEL), 1.0),
        "norm1_g": gain(ks[2], (Dp, D_MODEL)),
        "w_in": nrm(ks[3], (Dp, D_MODEL, IN_WIDTH), D_MODEL ** -0.5),
        "q_norm_g": gain(ks[4], (Dp, ATT_SUB_DIM)),
        "k_norm_g": gain(ks[5], (Dp, ATT_SUB_DIM)),
        "lambda_q1": nrm(ks[6], (Dp, ATT_SUB_DIM), 0.1),
        "lambda_k1": nrm(ks[7], (Dp, ATT_SUB_DIM), 0.1),
        "lambda_q2": nrm(ks[8], (Dp, ATT_SUB_DIM), 0.1),
        "lambda_k2": nrm(ks[9], (Dp, ATT_SUB_DIM), 0.1),
        "subln_g": gain(ks[10], (Dp, ATT_V_DIM)),
        "w_attn_branch": nrm(ks[11], (Dp, ATT_WIDTH, D_MODEL), ATT_WIDTH ** -0.5),
        "ssm_a_re": -0.5 + 0.01 * jax.random.normal(ks[12], (Dp, SSM_GROUPS, SSM_STATE), f32),
        "ssm_a_im": a_im_base + 0.01 * jax.random.normal(ks[13], (Dp, SSM_GROUPS, SSM_STATE), f32),
        "ssm_log_dt": jax.random.uniform(ks[14], (Dp, SSM_GROUPS), f32, math.log(0.001), math.log(0.1)),
        "ssm_b_re": nrm(ks[15], (Dp, SSM_GROUPS, SSM_STATE, SSM_GROUP), (2 * SSM_GROUP) ** -0.5),
        "ssm_b_im": nrm(ks[16], (Dp, SSM_GROUPS, SSM_STATE, SSM_GROUP), (2 * SSM_GROUP) ** -0.5),
        "ssm_c_re": nrm(ks[17], (Dp, SSM_GROUPS, SSM_GROUP, SSM_STATE), (2 * SSM_STATE) ** -0.5),
        "ssm_c_im": nrm(ks[18], (Dp, SSM_GROUPS, SSM_GROUP, SSM_STATE), (2 * SSM_STATE) ** -0.5),
        "ssm_d": nrm(ks[19], (Dp, SSM_WIDTH), 1.0),
        "w_glu": nrm(ks[20], (Dp, SSM_WIDTH, 2 * D_MODEL), SSM_WIDTH ** -0.5),
        "w_out": nrm(ks[21], (Dp, D_MODEL, D_MODEL), D_MODEL ** -0.5),
        "norm2_g": gain(ks[22], (Dp, D_MODEL)),
        "peer_w_q": nrm(ks[23], (Dp, D_MODEL, PEER_HEADS * PEER_QDIM), D_MODEL ** -0.5),
        "peer_k1": nrm(ks[24], (Dp, PEER_HEADS, PEER_KEYS, PEER_HALF), PEER_HALF ** -0.5),
        "peer_k2": nrm(ks[25], (Dp, PEER_HEADS, PEER_KEYS, PEER_HALF), PEER_HALF ** -0.5),
        "peer_u": nrm(ks[26], (Dp, PEER_EXPERTS, D_MODEL), D_MODEL ** -0.5),
        "peer_v": nrm(ks[27], (Dp, PEER_EXPERTS, D_MODEL), PEER_HEADS ** -0.5),
    }


def reference(x, meta_tokens, norm1_g, w_in, q_norm_g, k_norm_g, lambda_q1, lambda_k1, lambda_q2, lambda_k2,
              subln_g, w_attn_branch, ssm_a_re, ssm_a_im, ssm_log_dt, ssm_b_re, ssm_b_im, ssm_c_re, ssm_c_im,
              ssm_d, w_glu, w_out, norm2_g, peer_w_q, peer_k1, peer_k2, peer_u, peer_v):
    B, S = x.shape[0], x.shape[1]
    Lp = -(-(S + N_META) // BLOCK) * BLOCK
    meta = jnp.broadcast_to(meta_tokens.astype(x.dtype)[None], (B, N_META, D_MODEL))
    pad = jnp.zeros((B, Lp - S - N_META, D_MODEL), x.dtype)
    h = jnp.concatenate([meta, x, pad], axis=1)
    pos = jnp.arange(Lp)
    for l in range(DEPTH):
        lam_init = 0.8 - 0.6 * math.exp(-0.3 * l)
        hn = rms_norm(h, norm1_g[l])
        proj = hn @ w_in[l]
        q, k, v, u, gate_a, gate_b = jnp.split(proj, IN_SPLITS, axis=-1)
        q = q.reshape(B, Lp, ATT_HEADS, 2, ATT_SUB_DIM)
        k = k.reshape(B, Lp, ATT_HEADS, 2, ATT_SUB_DIM)
        v = v.reshape(B, Lp, ATT_HEADS, ATT_V_DIM)
        q = rotary(rms_norm(q, q_norm_g[l]), pos)
        k = rotary(rms_norm(k, k_norm_g[l]), pos)
        lam = (jnp.exp(jnp.sum(lambda_q1[l].astype(jnp.float32) * lambda_k1[l].astype(jnp.float32)))
               - jnp.exp(jnp.sum(lambda_q2[l].astype(jnp.float32) * lambda_k2[l].astype(jnp.float32)))
               + lam_init)
        att = diff_attention(q, k, v, lam)
        att = rms_norm(att, subln_g[l]) * (1.0 - lam_init)
        y_a = att.reshape(B, Lp, ATT_WIDTH) @ w_attn_branch[l]
        y_s = s5_ssm(u, ssm_a_re[l], ssm_a_im[l], ssm_log_dt[l], ssm_b_re[l], ssm_b_im[l],
                     ssm_c_re[l], ssm_c_im[l], ssm_d[l])
        glu = jax.nn.gelu(y_s, approximate=False) @ w_glu[l]
        y_b = glu[..., :D_MODEL] * jax.nn.sigmoid(glu[..., D_MODEL:])
        mix = jax.nn.sigmoid(gate_a) * y_a + jax.nn.sigmoid(gate_b) * y_b
        h = h + mix @ w_out[l]
        h = h + peer(rms_norm(h, norm2_g[l]), peer_w_q[l], peer_k1[l], peer_k2[l], peer_u[l], peer_v[l])
    return h[:, N_META:N_META + S]
```

```python
import contextlib
import math
import numpy as np
import concourse.bass as bass
import concourse.mybir as mybir
from concourse.bass_utils import run_bass_kernel_spmd

F32 = mybir.dt.float32
BF16 = mybir.dt.bfloat16
I32 = mybir.dt.int32
ALU = mybir.AluOpType
AF = mybir.ActivationFunctionType
AX = mybir.AxisListType

ENGS = ["pe", "act", "dve", "pool", "sp"]
NCORES = 8
LP = 4224
NT = 2 * LP
EPS = 1e-6
LAM_INIT = 0.2
TWO_PI = 2.0 * math.pi
import os
DEBUG = bool(os.environ.get('KDEBUG'))


class FW:
    def __init__(self, nc, stack):
        self.nc = nc
        self.stack = stack
        self.ops = {e: [] for e in ENGS}
        self.ecnt = {e: 0 for e in ENGS}
        self.seen = {e: {} for e in ENGS}
        self.lastw = {}
        self.readers = {}
        self.dcnt = {}
        self.semobj = {}
        for e in ENGS:
            self.semobj["es_" + e] = stack.enter_context(nc.semaphore("es_" + e))

    def _slot(self, name):
        s = "ds_" + name
        if s not in self.semobj:
            self.semobj[s] = self.stack.enter_context(self.nc.semaphore(s))
            self.dcnt[s] = 0
        return s

    def _deps(self, reads, writes):
        ev = []
        for k in reads:
            if k in self.lastw:
                ev.append(self.lastw[k])
        for k in writes:
            if k in self.lastw:
                ev.append(self.lastw[k])
            ev.extend(self.readers.get(k, ()))
        return ev

    def _filter(self, e, evs):
        need = {}
        for (s, v) in evs:
            if s == "es_pe" and e == "pe":
                continue
            if v > need.get(s, 0):
                need[s] = v
        out = []
        for s, v in need.items():
            if self.seen[e].get(s, 0) >= v:
                continue
            self.seen[e][s] = v
            out.append((s, v))
        return out

    def _record(self, ev, reads, writes):
        for k in writes:
            self.lastw[k] = ev
            self.readers[k] = []
        for k in reads:
            self.readers.setdefault(k, []).append(ev)

    def op(self, e, fn, reads=(), writes=(), inc=True):
        waits = self._filter(e, self._deps(reads, writes))
        ev = ("es_" + e, self.ecnt[e] + 1)
        if inc:
            self.ecnt[e] += 1
        self.ops[e].append((waits, fn, ("es_" + e, 1) if inc else None))
        self._record(ev, reads, writes)

    def dma(self, q, out, in_, reads=(), writes=(), slot=None):
        s = self._slot(slot)
        waits = self._filter(q, self._deps(reads, writes))
        self.dcnt[s] += 16
        ev = (s, self.dcnt[s])
        self.ops[q].append((waits, lambda eng: eng.dma_start(out=out, in_=in_), (s, 16)))
        self._record(ev, reads, writes)

    def raw(self, e, fn, reads=(), writes=(), slot=None, incv=1):
        s = self._slot(slot)
        waits = self._filter(e, self._deps(reads, writes))
        self.dcnt[s] += incv
        ev = (s, self.dcnt[s])
        self.ops[e].append((waits, fn, (s, incv)))
        self._record(ev, reads, writes)

    def _all_events(self):
        evs = list(self.lastw.values())
        for l in self.readers.values():
            evs.extend(l)
        for en in ENGS:
            if self.ecnt[en] > 0:
                evs.append(("es_" + en, self.ecnt[en]))
        return evs

    def barrier(self, engines=ENGS):
        evs = self._all_events()
        for e in engines:
            waits = self._filter(e, evs)
            if waits:
                self.ops[e].append((waits, None, None))
        if len(engines) == len(ENGS):
            self.lastw = {}
            self.readers = {}

    def replay(self):
        fw = self

        def run(eng, lst):
            for waits, fn, inc in lst:
                for (s, v) in waits:
                    eng.wait_ge(fw.semobj[s], v)
                if fn is None:
                    continue
                ins = fn(eng)
                if inc is not None:
                    ins.then_inc(fw.semobj[inc[0]], inc[1])

        with self.nc.Block() as block:
            @block.tensor
            def _(eng):
                run(eng, fw.ops["pe"])

            @block.scalar
            def _(eng):
                run(eng, fw.ops["act"])

            @block.vector
            def _(eng):
                run(eng, fw.ops["dve"])

            @block.gpsimd
            def _(eng):
                run(eng, fw.ops["pool"])

            @block.sync
            def _(eng):
                run(eng, fw.ops["sp"])


def _reshape(ap, shape):
    if len(shape) == 2:
        return ap
    if len(shape) == 3:
        return ap.rearrange("p (a b) -> p a b", a=shape[1])
    if len(shape) == 4:
        return ap.rearrange("p (a b c) -> p a b c", a=shape[1], b=shape[2])
    raise ValueError(shape)


class Arena:
    def __init__(self, t, ncols):
        self.t = t
        self.n = ncols
        self.off = 0

    def reset(self):
        self.off = 0

    def _take(self, n32):
        c0 = self.off
        self.off += n32
        assert self.off <= self.n, ("arena overflow", self.off, self.n)
        return self.t[:, c0:c0 + n32]

    def f32(self, shape):
        n = int(np.prod(shape[1:]))
        return _reshape(self._take(n), shape)

    def i32(self, shape):
        n = int(np.prod(shape[1:]))
        return _reshape(self._take(n).bitcast(I32), shape)

    def bf16(self, shape):
        n = int(np.prod(shape[1:]))
        n32 = (n + 1) // 2
        return _reshape(self._take(n32).bitcast(BF16)[:, 0:n], shape)


class K:
    def __init__(self, fw):
        self.fw = fw

    def mm(self, out, lhsT, rhs, start, stop, r, w, inc=True):
        self.fw.op("pe", lambda e: e.matmul(out, lhsT=lhsT, rhs=rhs, start=start, stop=stop), r, w, inc)

    def tr(self, out, in_, ident, r, w, inc=True):
        self.fw.op("pe", lambda e: e.transpose(out, in_, ident), r, w, inc)

    def act(self, out, in_, func, r, w, scale=None, bias=None, accum=None):
        kw = {}
        if scale is not None:
            kw["scale"] = scale
        if bias is not None:
            kw["bias"] = bias
        if accum is not None:
            kw["accum_out"] = accum
        self.fw.op("act", lambda e: e.activation(out=out, in_=in_, func=func, **kw), r, w)

    def tt(self, eng, out, in0, in1, op, r, w):
        self.fw.op(eng, lambda e: e.tensor_tensor(out=out, in0=in0, in1=in1, op=op), r, w)

    def ts(self, eng, out, in0, s1, op0, r, w, s2=None, op1=None):
        if s2 is None:
            self.fw.op(eng, lambda e: e.tensor_scalar(out=out, in0=in0, scalar1=s1, scalar2=None, op0=op0), r, w)
        else:
            self.fw.op(eng, lambda e: e.tensor_scalar(out=out, in0=in0, scalar1=s1, scalar2=s2, op0=op0, op1=op1), r, w)

    def stt(self, out, in0, scalar, in1, op0, op1, r, w):
        self.fw.op("dve", lambda e: e.scalar_tensor_tensor(out=out, in0=in0, scalar=scalar, in1=in1, op0=op0, op1=op1), r, w)

    def copy(self, eng, out, in_, r, w):
        if eng == "act":
            self.fw.op("act", lambda e: e.activation(out=out, in_=in_, func=AF.Copy), r, w)
        else:
            self.fw.op(eng, lambda e: e.tensor_copy(out=out, in_=in_), r, w)

    def recip(self, out, in_, r, w):
        self.fw.op("dve", lambda e: e.reciprocal(out=out, in_=in_), r, w)

    def memset(self, eng, ap, val, w):
        self.fw.op(eng, lambda e: e.memset(ap, val), (), w)

    def rsqrt(self, out, in_, scale, epsc, r, w):
        self.act(out, in_, AF.Sqrt, r, w, scale=scale, bias=epsc)
        self.recip(out, out, w, w)


def build_program():
    nc = bass.Bass("TRN2", target_bir_lowering=False)
    I = {}

    def inp(name, shape):
        I[name] = nc.dram_tensor(name, list(shape), F32, kind="ExternalInput").ap()

    inp("hT", [11, 128, 16 * 384])
    inp("vvalid", [128, 33])
    inp("xrow", [1024, 2048])
    inp("xTb", [2048, 1024])
    inp("w_a", [4, 128, 16 * 1024])
    inp("w_gate", [8, 128, 16 * 512])
    inp("w_ab", [4, 128, 8 * 512])
    inp("w_glu", [8, 128, 8 * 512])
    inp("w_out", [4, 128, 16 * 512])
    inp("w_q", [4, 128, 16 * 512])
    inp("ut", [32, 128, 16 * 512])
    inp("vv", [32, 128, 4 * 2048])
    inp("kt", [128, 16 * 128])
    inp("g1", [128, 16])
    inp("g2", [128, 16])
    inp("gqk", [128, 2])
    inp("lamv", [128, 4 * 64])
    inp("gsub", [128, 128])
    inp("ropec", [128, LP])
    inp("ropes", [128, LP])
    inp("cmat", [128, 4 * 128])
    inp("masks", [128, 4 * 512])
    inp("sa", [8, 128, 2 * 64])
    inp("sb", [8, 128, 2 * 64])
    inp("sldt", [8, 128, 1])
    inp("sS", [8, 128, 3 * 8])
    inp("sc", [8, 128, 2 * 128])
    inp("sd", [8, 128, 1])
    inp("smask", [128, 8 + 8 * 128 + 1])
    inp("iota", [128, LP])
    out_d = nc.dram_tensor("out", [1024, 2048], F32, kind="ExternalOutput").ap()
    gsc = nc.dram_tensor("gsc", [2048, 512], F32).ap()
    gsc_b = gsc.bitcast(BF16)

    with contextlib.ExitStack() as st:
        fw = FW(nc, st)
        k = K(fw)
        ARENA_COLS = 53200
        arena_t = st.enter_context(nc.sbuf_tensor("arena", [128, ARENA_COLS], F32))
        ar = Arena(arena_t, ARENA_COLS)
        ps = [st.enter_context(nc.psum_tensor("bank%d" % i, [128, 512], F32)) for i in range(8)]
        psk = ["ps%d" % i for i in range(8)]

        def psbf(i):
            return ps[i][:, :].bitcast(BF16)

        cm = ar.bf16([128, 4, 128])
        ident, perm, bones, ones = cm[:, 0, :], cm[:, 1, :], cm[:, 2, :], cm[:, 3, :]
        fw.dma("pool", cm, I["cmat"].rearrange("p (a b) -> p a b", a=4), writes=["cm"], slot="s_cm")
        msk = ar.bf16([128, 4, 512])
        fw.dma("pool", msk, I["masks"].rearrange("p (a b) -> p a b", a=4), writes=["msk"], slot="s_msk")
        g1 = ar.f32([128, 16]); fw.dma("sp", g1, I["g1"], writes=["g1"], slot="s_g1")
        gqk = ar.f32([128, 2]); fw.dma("sp", gqk, I["gqk"], writes=["gqk"], slot="s_gqk")
        lamv = ar.f32([128, 4, 64])
        fw.dma("sp", lamv, I["lamv"].rearrange("p (a b) -> p a b", a=4), writes=["lamv"], slot="s_lamv")
        gsub = ar.f32([128, 128]); fw.dma("sp", gsub, I["gsub"], writes=["gsub"], slot="s_gsub")
        vval = ar.f32([128, 33]); fw.dma("sp", vval, I["vvalid"], writes=["vval"], slot="s_vval")
        smk = ar.f32([128, 8 + 1024 + 1]); fw.dma("sp", smk, I["smask"], writes=["smk"], slot="s_smk")
        rowmask = smk[:, 0:8]
        colmask = smk[:, 8:8 + 1024].rearrange("p (a b) -> p a b", a=8)
        sgn = smk[:, 1032:1033]
        iota = ar.f32([128, LP]); fw.dma("sp", iota, I["iota"], writes=["iota"], slot="s_iota")
        epsc = ar.f32([128, 1])
        k.memset("dve", epsc, EPS, ["epsc"])
        onec = ar.f32([128, 1])
        k.memset("dve", onec, 1.0, ["onec"])
        magc = ar.f32([128, 2])
        k.memset("dve", magc[:, 0:1], 12582912.0, ["magc"])
        k.memset("dve", magc[:, 1:2], -12582912.0, ["magc"])
        k.ts("dve", gsub, gsub, 1.0 - LAM_INIT, ALU.mult, ["gsub"], ["gsub"])
        lp = ar.f32([128, 2, 64]); lsum = ar.f32([128, 2]); lexp = ar.f32([128, 2]); neglam = ar.f32([128, 1])
        k.tt("dve", lp[:, 0, :], lamv[:, 0, :], lamv[:, 1, :], ALU.mult, ["lamv"], ["lp"])
        k.tt("dve", lp[:, 1, :], lamv[:, 2, :], lamv[:, 3, :], ALU.mult, ["lamv"], ["lp"])
        fw.op("dve", lambda e: e.tensor_reduce(out=lsum, in_=lp, axis=AX.X, op=ALU.add), ["lp"], ["lsum"])
        k.act(lexp, lsum, AF.Exp, ["lsum"], ["lexp"])
        k.tt("dve", neglam, lexp[:, 1:2], lexp[:, 0:1], ALU.subtract, ["lexp"], ["neglam"])
        k.ts("dve", neglam, neglam, -LAM_INIT, ALU.add, ["neglam"], ["neglam"])
        rstd_all = ar.f32([128, LP])
        mark_pass = ar.off
        hT = I["hT"]
        WIN0 = LP - 1024
        MAGIC = 12582912.0

        for p in range(4):
            fw.barrier()
            ar.off = mark_pass
            Wa = ar.bf16([128, 16, 1024])
            fw.dma("pool", Wa, I["w_a"][p].rearrange("p (k f) -> p k f", k=16), writes=["Wa"], slot="s_wa")
            for kk in range(16):
                k.ts("dve", Wa[:, kk, :], Wa[:, kk, :], g1[:, kk:kk + 1], ALU.mult, ["Wa", "g1"], ["Wa"])
            QT = ar.bf16([128, 2, 1152])
            KT = ar.bf16([128, 2, LP])
            UT = ar.bf16([128, 2, LP])
            Vtok = ar.bf16([128, 33, 2, 130])
            for hl in range(2):
                k.copy("dve", Vtok[:, :, hl, 128:129], vval.unsqueeze(2), ["vval"], ["Vtok"])
            mark_a = ar.off
            xbs = [ar.bf16([128, 16, 384]) for _ in range(2)]
            xsq = ar.bf16([128, 16, 384])
            rcs = [ar.f32([128, 384]) for _ in range(2)]
            rss = [ar.f32([128, 384]) for _ in range(2)]
            qs_ = [ar.f32([128, 384]) for _ in range(2)]; sqb_ = [ar.bf16([128, 384]) for _ in range(2)]
            rq_ = [ar.f32([128, 384]) for _ in range(2)]; qn_ = [ar.bf16([128, 384]) for _ in range(2)]
            t1_ = [ar.f32([128, 384]) for _ in range(2)]; t2_ = [ar.f32([128, 384]) for _ in range(2)]
            vsb_ = [ar.bf16([128, 384]) for _ in range(2)]
            fc = 0
            pbc = 0
            for ci in range(11):
                b = ci % 2
                col0 = ci * 384
                cols = slice(col0, col0 + 384)
                xb = xbs[b]
                xbk = "xb%d" % b
                fw.dma("pool", xb, hT[ci].rearrange("p (k t) -> p k t", k=16), writes=[xbk], slot=xbk)
                fw.dma("sp", rcs[b], I["ropec"][:, cols], writes=["rc%d" % b], slot="rc%d" % b)
                fw.dma("sp", rss[b], I["ropes"][:, cols], writes=["rs%d" % b], slot="rs%d" % b)
                rstd = rstd_all[:, cols]
                if p == 0:
                    k.act(xsq, xb, AF.Square, [xbk], ["xsq"])
                    for kk in range(16):
                        k.mm(ps[0][:, 0:384], ones, xsq[:, kk, :], kk == 0, kk == 15, ["cm", "xsq"], ["ps0"], inc=(kk == 15))
                    k.rsqrt(rstd, ps[0][:, 0:384], 1.0 / 2048, epsc, ["ps0", "epsc"], ["rstd"])
                fts = [("k", 0, 2), ("k", 1, 3), ("v", 0, 4), ("v", 1, 5), ("u", 0, 6), ("u", 1, 7)]
                if ci >= 8:
                    fts = [("q", 0, 0), ("q", 1, 1)] + fts
                for kind, hl, f in fts:
                    pb = 1 + pbc % 4
                    pbc += 1
                    pbk = psk[pb]
                    for kk in range(16):
                        k.mm(ps[pb][:, 0:384], Wa[:, kk, f * 128:(f + 1) * 128], xb[:, kk, :], kk == 0, kk == 15,
                             ["Wa", xbk], [pbk], inc=(kk == 15))
                    if kind in ("q", "k"):
                        gi = 0 if kind == "q" else 1
                        z = fc % 2
                        fc += 1
                        qs, sqb, rq, qn, t1, t2 = qs_[z], sqb_[z], rq_[z], qn_[z], t1_[z], t2_[z]
                        zs = str(z)
                        pn = 5 + z
                        k.tt("dve", qs, ps[pb][:, 0:384], rstd, ALU.mult, [pbk, "rstd"], ["qs" + zs])
                        k.act(sqb, qs, AF.Square, ["qs" + zs], ["sqb" + zs])
                        k.mm(ps[pn][:, 0:384], bones, sqb, True, True, ["cm", "sqb" + zs], [psk[pn]])
                        k.rsqrt(rq, ps[pn][:, 0:384], 1.0 / 64, epsc, [psk[pn], "epsc"], ["rq" + zs])
                        k.stt(qn, qs, gqk[:, gi:gi + 1], rq, ALU.mult, ALU.mult, ["qs" + zs, "gqk", "rq" + zs], ["qn" + zs])
                        k.mm(ps[pn][:, 0:384], perm, qn, True, True, ["cm", "qn" + zs], [psk[pn]])
                        k.tt("dve", t1, qn, rcs[b], ALU.mult, ["qn" + zs, "rc%d" % b], ["t1" + zs])
                        k.tt("dve", t2, ps[pn][:, 0:384], rss[b], ALU.mult, [psk[pn], "rs%d" % b], ["t2" + zs])
                        if kind == "q":
                            dst = QT[:, hl, (ci - 8) * 384:(ci - 8) * 384 + 384]
                            dk = "QT"
                        else:
                            dst = KT[:, hl, cols]
                            dk = "KT"
                        k.tt("dve", dst, t1, t2, ALU.add, ["t1" + zs, "t2" + zs], [dk])
                    elif kind == "v":
                        z = fc % 2
                        fc += 1
                        vsb = vsb_[z]
                        k.tt("dve", vsb, ps[pb][:, 0:384], rstd, ALU.mult, [pbk, "rstd"], ["vsb%d" % z])
                        p7 = psbf(7)
                        for j in range(3):
                            k.tr(p7[:, j * 128:(j + 1) * 128], vsb[:, j * 128:(j + 1) * 128], ident, ["vsb%d" % z, "cm"], ["ps7"])
                        k.copy("act", Vtok[:, ci * 3:ci * 3 + 3, hl, 0:128],
                               p7[:, 0:384].rearrange("p (a b) -> p a b", a=3), ["ps7"], ["Vtok"])
                    else:
                        k.tt("dve", UT[:, hl, cols], ps[pb][:, 0:384], rstd, ALU.mult, [pbk, "rstd"], ["UT"])

            fw.barrier()
            ar.off = mark_a
            Pb = [ar.bf16([128, 512]) for _ in range(2)]
            Asb = [ar.f32([128, 4, 128]) for _ in range(2)]
            rl = ar.f32([128, 1])
            attf = ar.f32([128, 4, 128])
            junk = ar.f32([128, 128])
            ss4 = ar.f32([128, 4])
            attn = ar.bf16([128, 4, 128])
            attT = [ar.bf16([128, 512]) for _ in range(2)]
            ai = 0
            for hl in range(2):
                for qc in range(2):
                    kd = 25 + 4 * qc
                    nkt = kd + 4
                    for sub in range(2):
                        pr = slice(sub * 64, sub * 64 + 64)
                        def emit_s(kt_):
                            sb_ = kt_ % 2
                            k.mm(ps[sb_][:, :], KT[pr, hl, kt_ * 128:kt_ * 128 + 128],
                                 QT[pr, hl, 128 + qc * 512:128 + qc * 512 + 512], True, True, ["KT", "QT"], [psk[sb_]])
                            k.act(Pb[sb_], ps[sb_][:, :], AF.Exp, [psk[sb_]], ["P%d" % sb_], scale=0.125)

                        emit_s(0)
                        for kt_ in range(nkt):
                            sb_ = kt_ % 2
                            if kt_ + 1 < nkt:
                                emit_s(kt_ + 1)
                            P = Pb[sb_]
                            pk = "P%d" % sb_
                            i = kt_ - kd
                            if i >= 0:
                                k.tt("dve", P, P, msk[:, i, :], ALU.mult, [pk, "msk"], [pk])
                            for j in range(4):
                                if kt_ <= kd + j:
                                    k.mm(ps[2 + j][:, 0:129], P[:, j * 128:(j + 1) * 128], Vtok[:, kt_, hl, 0:129],
                                         kt_ == 0, kt_ == kd + j, [pk, "Vtok"], [psk[2 + j]])
                        for j in range(4):
                            k.recip(rl, ps[2 + j][:, 128:129], [psk[2 + j]], ["rl"])
                            k.ts("dve", Asb[sub][:, j, :], ps[2 + j][:, 0:128], rl[:, 0:1], ALU.mult,
                                 [psk[2 + j], "rl"], ["A%d" % sub])
                    for j in range(4):
                        k.stt(attf[:, j, :], Asb[1][:, j, :], neglam[:, 0:1], Asb[0][:, j, :], ALU.mult, ALU.add,
                              ["A0", "A1", "neglam"], ["attf"])
                        k.act(junk, attf[:, j, :], AF.Square, ["attf"], ["junk", "ss4"], accum=ss4[:, j:j + 1])
                    k.rsqrt(ss4, ss4, 1.0 / 128, epsc, ["ss4", "epsc"], ["ss4"])
                    p6 = psbf(6)
                    for j in range(4):
                        k.stt(attn[:, j, :], attf[:, j, :], ss4[:, j:j + 1], gsub, ALU.mult, ALU.mult,
                              ["attf", "ss4", "gsub"], ["attn"])
                        k.tr(p6[:, j * 128:(j + 1) * 128], attn[:, j, :], ident, ["attn", "cm"], ["ps6"])
                    aT = attT[ai % 2]
                    ak = "attT%d" % (ai % 2)
                    ai += 1
                    k.copy("act", aT, p6[:, 0:512], ["ps6"], [ak])
                    r0 = (2 * p + hl) * 256
                    fw.dma("sp", gsc_b[r0:r0 + 128, qc * 512:qc * 512 + 512], aT, reads=[ak], writes=["gsc"], slot="gsc")

            fw.barrier()
            ar.off = mark_a
            sa = ar.f32([128, 2, 64]); sbb = ar.f32([128, 2, 64]); sldt = ar.f32([128, 1])
            sS = ar.f32([128, 3, 8]); scc = ar.f32([128, 2, 128]); sd = ar.f32([128, 1])
            dtc = ar.f32([128, 1])
            ard = ar.f32([128, 64]); aid = ar.f32([128, 64]); rr = ar.f32([128, 64])
            yy = ar.f32([128, 64]); kf = ar.f32([128, 64]); ff = ar.f32([128, 64])
            sn = ar.f32([128, 64]); hh = ar.f32([128, 64]); cs = ar.f32([128, 64])
            abr = ar.f32([128, 64]); abi = ar.f32([128, 64]); den = ar.f32([128, 64]); tq = ar.f32([128, 64])
            mr = ar.f32([128, 64]); mi = ar.f32([128, 64]); Bre = ar.f32([128, 64]); Bim = ar.f32([128, 64])
            Bpad = ar.bf16([128, 8, 128]); Bswp = ar.bf16([128, 8, 128])
            CAs = ar.f32([128, 128]); CBs = ar.f32([128, 128])
            CpA = ar.bf16([128, 8, 128]); CpB = ar.bf16([128, 8, 128])
            dtS = ar.f32([128, 8]); rS = ar.f32([128, 8]); thS = ar.f32([128, 8])
            carr = ar.f32([128, 8])
            TC = 512
            tys = [ar.f32([128, TC]) for _ in range(2)]; tkf = ar.f32([128, TC])
            COS = [ar.f32([128, TC]) for _ in range(2)]
            SIN = [ar.f32([128, TC]) for _ in range(2)]
            SSIN = [ar.f32([128, TC]) for _ in range(2)]
            th_ = ar.f32([128, TC])
            c1 = ar.f32([128, TC]); c2 = ar.f32([128, TC])
            w1s = [ar.f32([128, TC]) for _ in range(2)]; w2s = [ar.f32([128, TC]) for _ in range(2)]; zzs = [ar.f32([128, TC]) for _ in range(2)]
            u1 = [ar.bf16([128, TC]) for _ in range(2)]
            u2 = [ar.bf16([128, TC]) for _ in range(2)]
            ysf = ar.f32([128, TC])
            ysb = [ar.bf16([128, TC]) for _ in range(2)]
            W = ["ssmtmp"]
            ti = 0
            yi = 0
            for tl in range(2):
                T = 2 * p + tl
                fw.dma("sp", sa, I["sa"][T].rearrange("p (a b) -> p a b", a=2), writes=["sa"], slot="s_sa")
                fw.dma("sp", sbb, I["sb"][T].rearrange("p (a b) -> p a b", a=2), writes=["sbb"], slot="s_sb")
                fw.dma("sp", sldt, I["sldt"][T], writes=["sldt"], slot="s_sldt")
                fw.dma("sp", sS, I["sS"][T].rearrange("p (a b) -> p a b", a=3), writes=["sS"], slot="s_sS")
                fw.dma("sp", scc, I["sc"][T].rearrange("p (a b) -> p a b", a=2), writes=["scc"], slot="s_sc")
                fw.dma("sp", sd, I["sd"][T], writes=["sd"], slot="s_sd")
                k.act(dtc, sldt, AF.Exp, ["sldt"], W)
                k.ts("dve", ard, sa[:, 0, :], dtc[:, 0:1], ALU.mult, ["sa"] + W, W)
                k.ts("dve", aid, sa[:, 1, :], dtc[:, 0:1], ALU.mult, ["sa"] + W, W)
                k.act(rr, ard, AF.Exp, W, W)
                k.ts("dve", yy, aid, 1.0 / TWO_PI, ALU.mult, W, W)
                k.ts("dve", kf, yy, MAGIC, ALU.add, W, W)
                k.ts("dve", kf, kf, -MAGIC, ALU.add, W, W)
                k.tt("dve", ff, yy, kf, ALU.subtract, W, W)
                k.act(sn, ff, AF.Sin, W, W, scale=TWO_PI)
                k.act(hh, ff, AF.Sin, W, W, scale=math.pi)
                k.tt("dve", hh, hh, hh, ALU.mult, W, W)
                k.ts("dve", cs, hh, -2.0, ALU.mult, W, W, s2=1.0, op1=ALU.add)
                k.tt("dve", abr, rr, cs, ALU.mult, W, W)
                k.ts("dve", abr, abr, -1.0, ALU.add, W, W)
                k.tt("dve", abi, rr, sn, ALU.mult, W, W)
                k.tt("dve", den, sa[:, 0, :], sa[:, 0, :], ALU.mult, ["sa"] + W, W)
                k.tt("dve", tq, sa[:, 1, :], sa[:, 1, :], ALU.mult, ["sa"] + W, W)
                k.tt("dve", den, den, tq, ALU.add, W, W)
                k.recip(den, den, W, W)
                k.tt("dve", mr, abr, sa[:, 0, :], ALU.mult, ["sa"] + W, W)
                k.tt("dve", tq, abi, sa[:, 1, :], ALU.mult, ["sa"] + W, W)
                k.tt("dve", mr, mr, tq, ALU.add, W, W)
                k.tt("dve", mr, mr, den, ALU.mult, W, W)
                k.tt("dve", mi, abi, sa[:, 0, :], ALU.mult, ["sa"] + W, W)
                k.tt("dve", tq, abr, sa[:, 1, :], ALU.mult, ["sa"] + W, W)
                k.tt("dve", mi, mi, tq, ALU.subtract, W, W)
                k.tt("dve", mi, mi, den, ALU.mult, W, W)
                k.tt("dve", Bre, mr, sbb[:, 0, :], ALU.mult, ["sbb"] + W, W)
                k.tt("dve", tq, mi, sbb[:, 1, :], ALU.mult, ["sbb"] + W, W)
                k.tt("dve", Bre, Bre, tq, ALU.subtract, W, W)
                k.tt("dve", Bim, mr, sbb[:, 1, :], ALU.mult, ["sbb"] + W, W)
                k.tt("dve", tq, mi, sbb[:, 0, :], ALU.mult, ["sbb"] + W, W)
                k.tt("dve", Bim, Bim, tq, ALU.add, W, W)
                rmb = rowmask.unsqueeze(2).to_broadcast([128, 8, 64])
                breb = Bre.unsqueeze(1).to_broadcast([128, 8, 64])
                bimb = Bim.unsqueeze(1).to_broadcast([128, 8, 64])
                k.tt("dve", Bpad[:, :, 0:64], breb, rmb, ALU.mult, ["smk"] + W, ["Bpad"])
                k.tt("dve", Bpad[:, :, 64:128], bimb, rmb, ALU.mult, ["smk"] + W, ["Bpad"])
                k.tt("dve", Bswp[:, :, 0:64], bimb, rmb, ALU.mult, ["smk"] + W, ["Bpad"])
                k.ts("dve", tq, Bre, -1.0, ALU.mult, W, W)
                k.tt("dve", Bswp[:, :, 64:128], tq.unsqueeze(1).to_broadcast([128, 8, 64]), rmb, ALU.mult, ["smk"] + W, ["Bpad"])
                k.ts("dve", CAs, scc[:, 0, :], sgn, ALU.mult, ["scc", "smk"], W)
                k.ts("dve", CBs, scc[:, 1, :], -1.0, ALU.mult, ["scc"], W)
                k.tt("dve", CpA, CAs.unsqueeze(1).to_broadcast([128, 8, 128]), colmask, ALU.mult, ["smk"] + W, ["Cpad"])
                k.tt("dve", CpB, CBs.unsqueeze(1).to_broadcast([128, 8, 128]), colmask, ALU.mult, ["smk"] + W, ["Cpad"])
                k.act(dtS, sS[:, 2, :], AF.Exp, ["sS"], ["dtS"])
                k.tt("dve", rS, sS[:, 0, :], dtS, ALU.mult, ["sS", "dtS"], ["rS"])
                k.act(rS, rS, AF.Exp, ["rS"], ["rS"])
                k.tt("dve", thS, sS[:, 1, :], dtS, ALU.mult, ["sS", "dtS"], ["thS"])
                k.ts("dve", thS, thS, 1.0 / TWO_PI, ALU.mult, ["thS"], ["thS"])
                sunits = [(tc, g) for tc in range(9) for g in range(8)]

                def dims(tc):
                    n = TC if tc < 8 else LP - 8 * TC
                    return n, tc * TC

                def emit_tab1a(ui):
                    tc, g = sunits[ui]
                    n, p0_ = dims(tc)
                    cols = slice(p0_, p0_ + n)
                    k.act(tkf[:, 0:n], iota[:, cols], AF.Identity, ["iota", "thS", "magc"], ["tkf"], scale=thS[:, g:g + 1], bias=magc[:, 0:1])
                    k.act(tkf[:, 0:n], tkf[:, 0:n], AF.Identity, ["tkf", "magc"], ["tkf"], bias=magc[:, 1:2])

                def emit_tab1b(ui):
                    tc, g = sunits[ui]
                    n, p0_ = dims(tc)
                    cols = slice(p0_, p0_ + n)
                    tyb = tys[ui % 2]
                    k.ts("pool", tyb[:, 0:n], iota[:, cols], thS[:, g:g + 1], ALU.mult, ["iota", "thS"], ["ty%d" % (ui % 2)], s2=0.0, op1=ALU.add)
                    k.tt("pool", tyb[:, 0:n], tyb[:, 0:n], tkf[:, 0:n], ALU.subtract, ["ty%d" % (ui % 2), "tkf"], ["ty%d" % (ui % 2)])

                def emit_tab2(ui):
                    tc, g = sunits[ui]
                    n, p0_ = dims(tc)
                    tb = ui % 2
                    tk = "tab%d" % tb
                    tyb = tys[tb]
                    k.act(SIN[tb][:, 0:n], tyb[:, 0:n], AF.Sin, ["ty%d" % tb], [tk], scale=TWO_PI)
                    k.act(th_[:, 0:n], tyb[:, 0:n], AF.Sin, ["ty%d" % tb], ["th_"], scale=math.pi)
                    k.act(th_[:, 0:n], th_[:, 0:n], AF.Square, ["th_"], ["th_"])
                    k.act(COS[tb][:, 0:n], th_[:, 0:n], AF.Identity, ["th_", "onec"], [tk], scale=-2.0, bias=onec)

                NS = len(sunits)
                emit_tab1a(0); emit_tab1b(0); emit_tab2(0)
                emit_tab1a(1); emit_tab1b(1)
                for ui in range(NS):
                    tc, g = sunits[ui]
                    n, p0_ = dims(tc)
                    cols = slice(p0_, p0_ + n)
                    outp = tc >= 6
                    tb = ui % 2
                    tk = "tab%d" % tb
                    w1, w2, zz = w1s[tb], w2s[tb], zzs[tb]
                    wk1, wk2, zk = "w1%d" % tb, "w2%d" % tb, "zz%d" % tb
                    b1, b2 = (0, 1) if tb == 0 else (3, 4)
                    k.mm(ps[b1][:, 0:n], Bpad[:, g, :], UT[:, tl, cols], True, True, ["Bpad", "UT"], [psk[b1]])
                    k.mm(ps[b2][:, 0:n], Bswp[:, g, :], UT[:, tl, cols], True, True, ["Bpad", "UT"], [psk[b2]])
                    if ui + 1 < NS:
                        emit_tab2(ui + 1)
                    if ui + 2 < NS:
                        emit_tab1a(ui + 2)
                    k.tt("dve", w1[:, 0:n], ps[b1][:, 0:n], COS[tb][:, 0:n], ALU.mult, [psk[b1], tk], [wk1])
                    k.tt("dve", w2[:, 0:n], ps[b2][:, 0:n], SIN[tb][:, 0:n], ALU.mult, [psk[b2], tk], [wk2])
                    k.tt("dve", w1[:, 0:n], w1[:, 0:n], w2[:, 0:n], ALU.add, [wk1, wk2], [wk1])
                    rsb = rS[:, g:g + 1].to_broadcast([128, n])
                    init = 0.0 if tc == 0 else carr[:, g:g + 1]

                    def scan(e, rsb=rsb, init=init, n=n, zz=zz, w1=w1):
                        return e.tensor_tensor_scan(out=zz[:, 0:n], data0=rsb, data1=w1[:, 0:n], initial=init,
                                                    op0=ALU.mult, op1=ALU.add)
                    fw.op("dve", scan, ["rS", wk1, "carr"], [zk])
                    k.copy("dve", carr[:, g:g + 1], zz[:, n - 1:n], [zk], ["carr"])
                    if ui + 2 < NS:
                        emit_tab1b(ui + 2)
                    if outp:
                        ub = g % 2
                        k.tt("dve", u1[ub][:, 0:n], zz[:, 0:n], COS[tb][:, 0:n], ALU.mult, [zk, tk], ["u1%d" % ub])
                        k.tt("dve", u2[ub][:, 0:n], zz[:, 0:n], SIN[tb][:, 0:n], ALU.mult, [zk, tk], ["u2%d" % ub])
                        k.mm(ps[2][:, 0:n], CpA[:, g, :], u1[ub][:, 0:n], g == 0, False, ["Cpad", "u1%d" % ub], ["ps2"])
                        k.mm(ps[2][:, 0:n], CpB[:, g, :], u2[ub][:, 0:n], False, g == 7, ["Cpad", "u2%d" % ub], ["ps2"])
                    if outp and g == 7:
                        k.stt(ysf[:, 0:n], UT[:, tl, cols], sd[:, 0:1], ps[2][:, 0:n], ALU.mult, ALU.add,
                              ["UT", "sd", "ps2"], ["ysf"])
                        yb = ysb[yi % 2]
                        yk = "ysb%d" % (yi % 2)
                        yi += 1
                        k.act(yb[:, 0:n], ysf[:, 0:n], AF.Gelu, ["ysf"], [yk])
                        lo = max(p0_, WIN0)
                        r0 = T * 256 + 128
                        fw.dma("sp", gsc_b[r0:r0 + 128, lo - WIN0:p0_ + n - WIN0], yb[:, lo - p0_:n],
                               reads=[yk], writes=["gsc"], slot="gsc")
        if DEBUG:
            dbg = nc.dram_tensor("dbg_gsc", [2048, 512], F32, kind="ExternalOutput").ap()
            fw.dma("sp", dbg, gsc, reads=["gsc"], writes=["dbg"], slot="dbg")
        fw.barrier()

        ar.reset()
        cm = ar.bf16([128, 4, 128])
        ident = cm[:, 0, :]
        ar.off = mark_pb = ar.off
        zeroT = cm[:, 1, :]
        k.memset("dve", zeroT, 0.0, ["cm"])
        g1 = ar.f32([128, 16]); fw.dma("sp", g1, I["g1"], writes=["g1"], slot="s_g1")
        g2 = ar.f32([128, 16]); fw.dma("sp", g2, I["g2"], writes=["g2"], slot="s_g2")
        epsc = ar.f32([128, 1]); k.memset("dve", epsc, EPS, ["epsc"])
        KTb = ar.bf16([128, 16, 128])
        fw.dma("pool", KTb, I["kt"].rearrange("p (a b) -> p a b", a=16), writes=["KTb"], slot="s_kt")
        res = ar.f32([128, 4, 2048])
        T1 = ar.bf16([128, 16, 512])
        T2 = ar.bf16([128, 16, 512])
        big = ar._take(8192)
        mix = _reshape(big[:, 0:4096].bitcast(BF16), [128, 4, 2048])
        sfl = big
        s4 = big.rearrange("p (t f n) -> p t f n", t=4, f=16)
        Rw = [ar.bf16([128, 16, 512]) for _ in range(2)]
        Rv = [ar.bf16([128, 16, 512]) for _ in range(2)]
        scr = ar.bf16([128, 2048])
        ssq = ar.f32([128, 4]); r1 = ar.f32([128, 4]); r2 = ar.f32([128, 4])
        sab = ar.bf16([128, 512]); sbb2 = ar.bf16([128, 512]); sgb = ar.bf16([128, 512])
        ta = ar.f32([128, 512]); tb2 = ar.f32([128, 512])
        Dall = ar.f32([128, 8, 512])
        Dflat = Dall.rearrange("p h e -> p (h e)")
        cand = Dflat[:, 0:2048].rearrange("p (h e) -> p h e", h=8)
        tmp256 = Dflat[:, 2048:2304]
        tmp128 = Dflat[:, 2304:2432]
        v16 = Dflat[:, 2432:2688].rearrange("p (a b) -> p a b", a=16)
        top = ar.f32([128, 8, 16])
        tau = ar.f32([128, 4, 8]); nb = ar.f32([128, 4, 8])
        negm = ar.f32([128, 8]); Zs = ar.f32([128, 8]); lnZ = ar.f32([128, 8])
        junk16 = ar.f32([128, 16])
        gas = [ar.bf16([128, 512]) for _ in range(2)]
        wb = ar.bf16([128, 512])
        EX = ar.bf16([128, 8, 512])
        wTs = [ar.bf16([128, 4, 128]) for _ in range(2)]


        for hf in range(2):
            tsl = slice(hf * 512, hf * 512 + 512)
            fw.dma("sp", res, I["xrow"][tsl, :].rearrange("(t p) d -> p t d", p=128), writes=["res"], slot="res")
            fw.dma("pool", T1, I["xTb"][:, tsl].rearrange("(k p) t -> p k t", p=128), writes=["T1"], slot="T1")
            gv = gsc_b[:, tsl].rearrange("(r a p) t -> a p r t", a=2, p=128)
            for a in range(2):
                fw.dma("sp", T2[:, a * 8:a * 8 + 8, :], gv[a], reads=["gsc"], writes=["T2"], slot="T2")
            for tt in range(4):
                k.act(scr, res[:, tt, :], AF.Square, ["res"], ["scr", "ssq"], accum=ssq[:, tt:tt + 1])
            k.rsqrt(r1, ssq, 1.0 / 2048, epsc, ["ssq", "epsc"], ["r1"])
            k.tt("dve", T1, T1, g1.unsqueeze(2).to_broadcast([128, 16, 512]), ALU.mult, ["T1", "g1"], ["T1"])
            for n4 in range(4):
                c0 = n4 * 512
                fw.dma("pool", Rw[0], I["w_gate"][n4].rearrange("p (k f) -> p k f", k=16), writes=["Rw0"], slot="Rw0")
                fw.dma("pool", Rw[1], I["w_gate"][4 + n4].rearrange("p (k f) -> p k f", k=16), writes=["Rw1"], slot="Rw1")
                fw.dma("pool", Rv[0][:, 0:8, :], I["w_ab"][n4].rearrange("p (k f) -> p k f", k=8), writes=["Rv0"], slot="Rv0")
                fw.dma("pool", Rv[1][:, 0:8, :], I["w_glu"][n4].rearrange("p (k f) -> p k f", k=8), writes=["Rv1"], slot="Rv1")
                fw.dma("pool", Rv[1][:, 8:16, :], I["w_glu"][4 + n4].rearrange("p (k f) -> p k f", k=8), writes=["Rv1"], slot="Rv1")
                for tt in range(4):
                    tk_ = slice(tt * 128, tt * 128 + 128)
                    for kk in range(16):
                        k.mm(ps[0][:, :], T1[:, kk, tk_], Rw[0][:, kk, :], kk == 0, kk == 15, ["T1", "Rw0"], ["ps0"], inc=(kk == 15))
                    for kk in range(16):
                        k.mm(ps[1][:, :], T1[:, kk, tk_], Rw[1][:, kk, :], kk == 0, kk == 15, ["T1", "Rw1"], ["ps1"], inc=(kk == 15))
                    for kk in range(8):
                        k.mm(ps[2][:, :], T2[:, kk, tk_], Rv[0][:, kk, :], kk == 0, kk == 7, ["T2", "Rv0"], ["ps2"], inc=(kk == 7))
                    for kk in range(8):
                        k.mm(ps[3][:, :], T2[:, 8 + kk, tk_], Rv[1][:, kk, :], kk == 0, kk == 7, ["T2", "Rv1"], ["ps3"], inc=(kk == 7))
                    for kk in range(8):
                        k.mm(ps[4][:, :], T2[:, 8 + kk, tk_], Rv[1][:, 8 + kk, :], kk == 0, kk == 7, ["T2", "Rv1"], ["ps4"], inc=(kk == 7))
                    k.act(sab, ps[0][:, :], AF.Sigmoid, ["ps0", "r1"], ["sab"], scale=r1[:, tt:tt + 1])
                    k.act(sbb2, ps[1][:, :], AF.Sigmoid, ["ps1", "r1"], ["sbb2"], scale=r1[:, tt:tt + 1])
                    k.act(sgb, ps[4][:, :], AF.Sigmoid, ["ps4"], ["sgb"])
                    k.tt("dve", ta, ps[2][:, :], sab, ALU.mult, ["ps2", "sab"], ["ta"])
                    k.tt("dve", tb2, ps[3][:, :], sgb, ALU.mult, ["ps3", "sgb"], ["tb2"])
                    k.tt("pool", tb2, tb2, sbb2, ALU.mult, ["tb2", "sbb2"], ["tb2"])
                    k.tt("pool", mix[:, tt, c0:c0 + 512], ta, tb2, ALU.add, ["ta", "tb2"], ["big"])
            p7 = psbf(7)
            for tt in range(4):
                for half8 in range(2):
                    for kk in range(8):
                        kf_ = half8 * 8 + kk
                        k.tr(p7[:, kk * 128:(kk + 1) * 128], mix[:, tt, kf_ * 128:(kf_ + 1) * 128], ident, ["big", "cm"], ["ps7"])
                    k.copy("act", T2[:, half8 * 8:half8 * 8 + 8, tt * 128:(tt + 1) * 128],
                           p7.rearrange("p (a b) -> p a b", a=8), ["ps7"], ["T2"])
            for n4 in range(4):
                c0 = n4 * 512
                rb = n4 % 2
                fw.dma("pool", Rw[rb], I["w_out"][n4].rearrange("p (k f) -> p k f", k=16), writes=["Rw%d" % rb], slot="Rw%d" % rb)
                for tt in range(4):
                    tk_ = slice(tt * 128, tt * 128 + 128)
                    pb_ = tt % 2
                    for kk in range(16):
                        k.mm(ps[pb_][:, :], T2[:, kk, tk_], Rw[rb][:, kk, :], kk == 0, kk == 15, ["T2", "Rw%d" % rb], [psk[pb_]], inc=(kk == 15))
                    k.tt("dve", res[:, tt, c0:c0 + 512], ps[pb_][:, :], res[:, tt, c0:c0 + 512], ALU.add, [psk[pb_], "res"], ["res"])
            for tt in range(4):
                k.act(scr, res[:, tt, :], AF.Square, ["res"], ["scr", "ssq"], accum=ssq[:, tt:tt + 1])
            k.rsqrt(r2, ssq, 1.0 / 2048, epsc, ["ssq", "epsc"], ["r2"])
            for tt in range(4):
                k.copy("act", scr, res[:, tt, :], ["res"], ["scr"])
                for half8 in range(2):
                    for kk in range(8):
                        kf_ = half8 * 8 + kk
                        k.tr(p7[:, kk * 128:(kk + 1) * 128], scr[:, kf_ * 128:(kf_ + 1) * 128], ident, ["scr", "cm"], ["ps7"])
                    k.copy("dve", T1[:, half8 * 8:half8 * 8 + 8, tt * 128:(tt + 1) * 128],
                           p7.rearrange("p (a b) -> p a b", a=8), ["ps7"], ["T1"])
            k.tt("dve", T1, T1, g2.unsqueeze(2).to_broadcast([128, 16, 512]), ALU.mult, ["T1", "g2"], ["T1"])
            for wc in range(4):
                rb = wc % 2
                fw.dma("pool", Rw[rb], I["w_q"][wc].rearrange("p (k f) -> p k f", k=16), writes=["Rw%d" % rb], slot="Rw%d" % rb)
                for f4 in range(4):
                    ft = wc * 4 + f4
                    pb_ = f4 % 2
                    for kk in range(16):
                        k.mm(ps[pb_][:, :], Rw[rb][:, kk, f4 * 128:(f4 + 1) * 128], T1[:, kk, :], kk == 0, kk == 15, ["T1", "Rw%d" % rb], [psk[pb_]], inc=(kk == 15))
                    k.copy("act", T2[:, ft, :], ps[pb_][:, :], [psk[pb_]], ["T2"])
            for tt in range(4):
                tk_ = slice(tt * 128, tt * 128 + 128)
                for ft in range(16):
                    pb_ = 2 + ft // 4
                    k.mm(ps[pb_][:, (ft % 4) * 128:(ft % 4) * 128 + 128], T2[:, ft, tk_], KTb[:, ft, :], True, True, ["T2", "KTb"], [psk[pb_]])
                for f4 in range(4):
                    k.ts("dve", sfl[:, tt * 2048 + f4 * 512: tt * 2048 + f4 * 512 + 512], ps[2 + f4][:, :], r2[:, tt:tt + 1], ALU.mult, [psk[2 + f4], "r2"], ["big"])
                for ft in range(16):
                    sv = s4[:, tt, ft, :]
                    fw.op("dve", lambda e, sv=sv, ft=ft: e.max(out=v16[:, ft, 0:8], in_=sv), ["big"], ["Dall"])
                    fw.op("dve", lambda e, sv=sv, ft=ft: e.match_replace(out=tmp128, in_to_replace=v16[:, ft, 0:8], in_values=sv, imm_value=-1e30), ["big", "Dall"], ["Dall"])
                    fw.op("dve", lambda e, ft=ft: e.max(out=v16[:, ft, 8:16], in_=tmp128), ["Dall"], ["Dall"])
                v4 = v16.rearrange("p (h two) a -> p h two a", two=2)
                k.tt("pool", cand.rearrange("p h (a b) -> p h a b", a=16),
                     v4[:, :, 0, :].unsqueeze(3).to_broadcast([128, 8, 16, 16]),
                     v4[:, :, 1, :].unsqueeze(2).to_broadcast([128, 8, 16, 16]), ALU.add, ["Dall"], ["Dall"])
                for h in range(8):
                    fw.op("dve", lambda e, h=h: e.max(out=top[:, h, 0:8], in_=cand[:, h, :]), ["Dall"], ["top"])
                    fw.op("dve", lambda e, h=h: e.match_replace(out=tmp256, in_to_replace=top[:, h, 0:8], in_values=cand[:, h, :], imm_value=-1e30), ["Dall", "top"], ["Dall"])
                    fw.op("dve", lambda e, h=h: e.max(out=top[:, h, 8:16], in_=tmp256), ["Dall"], ["top"])
                k.copy("dve", tau[:, tt, :], top[:, :, 15], ["top"], ["tau"])
                k.ts("dve", negm, top[:, :, 0], -1.0, ALU.mult, ["top"], ["negm"])
                for h in range(8):
                    k.act(junk16, top[:, h, :], AF.Exp, ["top", "negm"], ["junk16", "Zs"], bias=negm[:, h:h + 1], accum=Zs[:, h:h + 1])
                k.act(lnZ, Zs, AF.Ln, ["Zs"], ["lnZ"])
                k.tt("dve", nb[:, tt, :], negm, lnZ, ALU.subtract, ["negm", "lnZ"], ["nb"])
            units = [(ec, tt) for ec in range(32) for tt in range(4)]
            NU = len(units)

            def emit_dma(ec):
                rb = ec % 2
                fw.dma("pool", Rw[rb], I["ut"][ec].rearrange("p (k e) -> p k e", k=16), writes=["Rw%d" % rb], slot="Rw%d" % rb)
                fw.dma("pool", Rv[rb], I["vv"][ec].rearrange("p (j b) -> p j b", j=16), writes=["Rv%d" % rb], slot="Rv%d" % rb)

            def emit_A(u):
                ec, tt = units[u]
                rb = ec % 2
                pa = u % 2
                tk_ = slice(tt * 128, tt * 128 + 128)
                for kk in range(16):
                    k.mm(ps[pa][:, :], T1[:, kk, tk_], Rw[rb][:, kk, :], kk == 0, kk == 15, ["T1", "Rw%d" % rb], [psk[pa]], inc=(kk == 15))

            def emit_G(u):
                ec, tt = units[u]
                pa = u % 2
                k.act(gas[pa], ps[pa][:, :], AF.Gelu, [psk[pa], "r2"], ["ga%d" % pa], scale=r2[:, tt:tt + 1])

            def emit_Vmm(u):
                ec, tt = units[u]
                rb = ec % 2
                Vb = Rv[rb].rearrange("p (j a) b -> p j (a b)", a=4)
                wTu = wTs[u % 2]
                for n4 in range(4):
                    for et in range(4):
                        k.mm(ps[3 + n4][:, :], wTu[:, et, :], Vb[:, et, n4 * 512:(n4 + 1) * 512], et == 0, et == 3, ["wT%d" % (u % 2), "Rv%d" % rb], [psk[3 + n4]], inc=(et == 3))

            def emit_Vadd(u):
                ec, tt = units[u]
                for n4 in range(4):
                    k.tt("dve", res[:, tt, n4 * 512:(n4 + 1) * 512], ps[3 + n4][:, :], res[:, tt, n4 * 512:(n4 + 1) * 512], ALU.add, [psk[3 + n4], "res"], ["res"])

            emit_dma(0)
            emit_A(0)
            emit_G(0)
            for u in range(NU):
                ec, tt = units[u]
                pa = u % 2
                if u + 1 < NU:
                    if units[u + 1][1] == 0:
                        emit_dma(units[u + 1][0])
                    emit_A(u + 1)
                if u >= 1:
                    emit_Vmm(u - 1)
                s5 = s4[:, tt, :, :].rearrange("p (h two) n -> p h two n", two=2)
                for hh in range(2):
                    hs = slice(hh * 4, hh * 4 + 4)
                    k.tt("pool", Dall[:, hs, :].rearrange("p h (a b) -> p h a b", a=4),
                         s5[:, hs, 0, ec * 4:ec * 4 + 4].unsqueeze(3).to_broadcast([128, 4, 4, 128]),
                         s5[:, hs, 1, :].unsqueeze(2).to_broadcast([128, 4, 4, 128]), ALU.add, ["big", "Dall"], ["D%d" % hh, "Dall"])
                for h in range(8):
                    ek = "EX%d" % h
                    dk_ = "D%d" % (h // 4)
                    k.act(EX[:, h, :], Dall[:, h, :], AF.Exp, [dk_, "nb"], [ek], bias=nb[:, tt, h:h + 1])
                    k.stt(EX[:, h, :], Dall[:, h, :], tau[:, tt, h:h + 1], EX[:, h, :], ALU.is_ge, ALU.mult,
                          [dk_, ek, "tau"], [ek])
                if u + 1 < NU:
                    emit_G(u + 1)
                for h in range(8):
                    k.mm(ps[2][:, :], ident, EX[:, h, :], h == 0, h == 7, ["EX%d" % h, "cm"], ["ps2"], inc=(h == 7))
                if u >= 1:
                    emit_Vadd(u - 1)
                k.tt("dve", wb, ps[2][:, :], gas[pa], ALU.mult, ["ps2", "ga%d" % pa], ["wb"])
                p7 = psbf(7)
                for et in range(4):
                    k.tr(p7[:, et * 128:(et + 1) * 128], wb[:, et * 128:(et + 1) * 128], ident, ["wb", "cm"], ["ps7"])
                k.copy("act", wTs[pa].rearrange("p a b -> p (a b)"), p7[:, 0:512], ["ps7"], ["wT%d" % pa])
            emit_Vmm(NU - 1)
            emit_Vadd(NU - 1)
            fw.dma("sp", out_d[tsl, :].rearrange("(t p) d -> p t d", p=128), res, reads=["res"], writes=["out"], slot="out")
        fw.barrier(["sp"])
        fw.replay()
    return nc


def _host_inputs(inp):
    f = np.float32
    x = np.asarray(inp["x"], f)
    meta = np.asarray(inp["meta_tokens"], f)
    w_in = np.asarray(inp["w_in"], f)[0]
    common = {}
    def tile_cols(w, cw):
        K, N = w.shape
        return np.ascontiguousarray(w.reshape(K // 128, 128, N // cw, cw).transpose(2, 1, 0, 3)).reshape(N // cw, 128, (K // 128) * cw)

    common["w_gate"] = tile_cols(w_in[:, 4096:8192], 512)
    common["w_ab"] = tile_cols(np.asarray(inp["w_attn_branch"], f)[0], 512)
    common["w_glu"] = tile_cols(np.asarray(inp["w_glu"], f)[0], 512)
    common["w_out"] = tile_cols(np.asarray(inp["w_out"], f)[0], 512)
    common["w_q"] = tile_cols(np.asarray(inp["peer_w_q"], f)[0], 512)
    pu = np.asarray(inp["peer_u"], f)[0]
    common["ut"] = np.ascontiguousarray(pu.reshape(32, 512, 16, 128).transpose(0, 3, 2, 1)).reshape(32, 128, 16 * 512)
    pv = np.asarray(inp["peer_v"], f)[0]
    common["vv"] = np.ascontiguousarray(pv.reshape(32, 4, 128, 2048).transpose(0, 2, 1, 3)).reshape(32, 128, 4 * 2048)
    k1 = np.asarray(inp["peer_k1"], f)[0]
    k2 = np.asarray(inp["peer_k2"], f)[0]
    kt = np.stack([k1, k2], axis=1)
    common["kt"] = np.ascontiguousarray(kt.transpose(3, 0, 1, 2).reshape(128, 16 * 128))
    common["g1"] = np.ascontiguousarray(np.asarray(inp["norm1_g"], f)[0].reshape(16, 128).T)
    common["g2"] = np.ascontiguousarray(np.asarray(inp["norm2_g"], f)[0].reshape(16, 128).T)
    gq = np.tile(np.asarray(inp["q_norm_g"], f)[0], 2)
    gk = np.tile(np.asarray(inp["k_norm_g"], f)[0], 2)
    common["gqk"] = np.ascontiguousarray(np.stack([gq, gk], axis=1))
    lamv = np.concatenate([np.asarray(inp[n], f)[0] for n in ("lambda_q1", "lambda_k1", "lambda_q2", "lambda_k2")])
    common["lamv"] = np.ascontiguousarray(np.broadcast_to(lamv[None, :], (128, 256)))
    common["gsub"] = np.ascontiguousarray(np.broadcast_to(np.asarray(inp["subln_g"], f)[0][None, :], (128, 128)))
    wa = np.zeros((4, 2048, 1024), f)
    for p in range(4):
        blocks = []
        for off in (0, 1024, 2048, 3072):
            for l in range(2):
                t = 2 * p + l
                blocks.append(w_in[:, off + t * 128: off + t * 128 + 128])
        wa[p] = np.concatenate(blocks, axis=1)
    common["w_a"] = np.stack([tile_cols(wa[p], 1024)[0] for p in range(4)], axis=0)
    fidx = np.arange(128) % 64
    inv_freq = np.power(np.float32(10000.0), -(np.arange(32, dtype=f) / np.float32(32))).astype(f)
    sgn_r = np.where(fidx < 32, -1.0, 1.0).astype(f)
    ident = np.eye(128, dtype=f)
    partner = np.where(fidx < 32, np.arange(128) + 32, np.arange(128) - 32)
    perm = np.zeros((128, 128), f)
    perm[partner, np.arange(128)] = 1.0
    bones = (np.arange(128)[:, None] // 64 == np.arange(128)[None, :] // 64).astype(f)
    ones = np.ones((128, 128), f)
    common["cmat"] = np.ascontiguousarray(np.stack([ident, perm, bones, ones], axis=1).reshape(128, 512))
    masks = np.zeros((128, 4, 512), f)
    for i in range(4):
        masks[:, i, :] = (i * 128 + np.arange(128)[:, None] <= np.arange(512)[None, :]).astype(f)
    common["masks"] = masks.reshape(128, 4 * 512)
    rowmask = (np.arange(128)[:, None] // 16 == np.arange(8)[None, :]).astype(f)
    colmask = np.broadcast_to((np.arange(8)[:, None] == np.arange(128)[None, :] // 16).astype(f)[None], (128, 8, 128))
    sgn = np.concatenate([np.ones(64, f), -np.ones(64, f)])[:, None]
    common["smask"] = np.ascontiguousarray(np.concatenate([rowmask, colmask.reshape(128, 1024), sgn], axis=1))
    common["iota"] = np.ascontiguousarray(np.broadcast_to(np.arange(LP, dtype=f)[None, :], (128, LP)))

    a_re = np.asarray(inp["ssm_a_re"], f)[0]; a_im = np.asarray(inp["ssm_a_im"], f)[0]
    ldt = np.asarray(inp["ssm_log_dt"], f)[0]
    b_re = np.asarray(inp["ssm_b_re"], f)[0]; b_im = np.asarray(inp["ssm_b_im"], f)[0]
    c_re = np.asarray(inp["ssm_c_re"], f)[0]; c_im = np.asarray(inp["ssm_c_im"], f)[0]
    d_sk = np.asarray(inp["ssm_d"], f)[0]
    sa = np.zeros((8, 128, 128), f); sb = np.zeros((8, 128, 128), f); sldt = np.zeros((8, 128, 1), f)
    sS = np.zeros((8, 128, 24), f); sc = np.zeros((8, 128, 256), f); sd = np.zeros((8, 128, 1), f)
    for T in range(8):
        gs = slice(8 * T, 8 * T + 8)
        sa[T] = np.concatenate([np.repeat(a_re[gs], 16, axis=0), np.repeat(a_im[gs], 16, axis=0)], axis=1)
        sb[T] = np.concatenate([b_re[gs].transpose(0, 2, 1).reshape(128, 64), b_im[gs].transpose(0, 2, 1).reshape(128, 64)], axis=1)
        sldt[T] = np.repeat(ldt[gs], 16)[:, None]
        aT = np.concatenate([a_re[gs].T, a_re[gs].T], axis=0)
        iT = np.concatenate([a_im[gs].T, a_im[gs].T], axis=0)
        lT = np.broadcast_to(ldt[gs][None, :], (128, 8))
        sS[T] = np.concatenate([aT, iT, lT], axis=1)
        crT = c_re[gs].transpose(2, 0, 1).reshape(64, 128)
        ciT = c_im[gs].transpose(2, 0, 1).reshape(64, 128)
        sc[T] = np.concatenate([np.concatenate([crT, ciT], axis=0), np.concatenate([ciT, crT], axis=0)], axis=1)
        sd[T] = d_sk[T * 128:(T + 1) * 128][:, None]
    common.update(sa=sa, sb=sb, sldt=sldt, sS=sS, sc=sc, sd=sd)

    maps = []
    for c in range(NCORES):
        m = dict(common)
        beta, jq = c // 4, c % 4
        npad = 3072 - 1024 * jq + 112
        hT = np.zeros((2048, LP), f)
        hT[:, npad:npad + 16] = meta.T
        hT[:, npad + 16:] = x[beta, 0:1024 * (jq + 1), :].T
        m["hT"] = np.ascontiguousarray(hT.reshape(16, 128, 11, 384).transpose(2, 1, 0, 3)).reshape(11, 128, 16 * 384)
        xs = x[beta, jq * 1024:(jq + 1) * 1024, :]
        m["xrow"] = np.ascontiguousarray(xs)
        m["xTb"] = np.ascontiguousarray(xs.T)
        pos = np.maximum(np.arange(LP) - npad, 0).astype(f)
        ang = pos[None, :] * inv_freq[fidx % 32][:, None]
        m["ropec"] = np.cos(ang).astype(f)
        m["ropes"] = (np.sin(ang) * sgn_r[:, None]).astype(f)
        valid = (np.arange(LP) >= npad).astype(f)
        m["vvalid"] = np.ascontiguousarray(valid.reshape(33, 128).T)
        maps.append(m)
    return maps


def kernel(**inputs):
    maps = _host_inputs(inputs)
    nc = build_program()
    res = run_bass_kernel_spmd(nc, maps, core_ids=list(range(NCORES)))
    out = np.zeros((2, 4096, 2048), np.float32)
    for c in range(NCORES):
        out[c // 4, (c % 4) * 1024:(c % 4 + 1) * 1024, :] = res.results[c]["out"]
    return out
```

```python
import contextlib
import math
import numpy as np
import concourse.bass as bass
import concourse.mybir as mybir
from concourse.bass_utils import run_bass_kernel_spmd

F32 = mybir.dt.float32
BF16 = mybir.dt.bfloat16
I32 = mybir.dt.int32
ALU = mybir.AluOpType
AF = mybir.ActivationFunctionType
AX = mybir.AxisListType

ENGS = ["pe", "act", "dve", "pool", "sp"]
NCORES = 8
LP = 4224
NT = 2 * LP
EPS = 1e-6
LAM_INIT = 0.2
TWO_PI = 2.0 * math.pi
import os
DEBUG = bool(os.environ.get('KDEBUG'))


class FW:
    def __init__(self, nc, stack):
        self.nc = nc
        self.stack = stack
        self.ops = {e: [] for e in ENGS}
        self.ecnt = {e: 0 for e in ENGS}
        self.seen = {e: {} for e in ENGS}
        self.lastw = {}
        self.readers = {}
        self.dcnt = {}
        self.semobj = {}
        for e in ENGS:
            self.semobj["es_" + e] = stack.enter_context(nc.semaphore("es_" + e))

    def _slot(self, name):
        s = "ds_" + name
        if s not in self.semobj:
            self.semobj[s] = self.stack.enter_context(self.nc.semaphore(s))
            self.dcnt[s] = 0
        return s

    def _deps(self, reads, writes):
        ev = []
        for k in reads:
            if k in self.lastw:
                ev.append(self.lastw[k])
        for k in writes:
            if k in self.lastw:
                ev.append(self.lastw[k])
            ev.extend(self.readers.get(k, ()))
        return ev

    def _filter(self, e, evs):
        need = {}
        for (s, v) in evs:
            if s == "es_pe" and e == "pe":
                continue
            if v > need.get(s, 0):
                need[s] = v
        out = []
        for s, v in need.items():
            if self.seen[e].get(s, 0) >= v:
                continue
            self.seen[e][s] = v
            out.append((s, v))
        return out

    def _record(self, ev, reads, writes):
        for k in writes:
            self.lastw[k] = ev
            self.readers[k] = []
        for k in reads:
            self.readers.setdefault(k, []).append(ev)

    def op(self, e, fn, reads=(), writes=(), inc=True):
        waits = self._filter(e, self._deps(reads, writes))
        ev = ("es_" + e, self.ecnt[e] + 1)
        if inc:
            self.ecnt[e] += 1
        self.ops[e].append((waits, fn, ("es_" + e, 1) if inc else None))
        self._record(ev, reads, writes)

    def dma(self, q, out, in_, reads=(), writes=(), slot=None):
        s = self._slot(slot)
        waits = self._filter(q, self._deps(reads, writes))
        self.dcnt[s] += 16
        ev = (s, self.dcnt[s])
        self.ops[q].append((waits, lambda eng: eng.dma_start(out=out, in_=in_), (s, 16)))
        self._record(ev, reads, writes)

    def raw(self, e, fn, reads=(), writes=(), slot=None, incv=1):
        s = self._slot(slot)
        waits = self._filter(e, self._deps(reads, writes))
        self.dcnt[s] += incv
        ev = (s, self.dcnt[s])
        self.ops[e].append((waits, fn, (s, incv)))
        self._record(ev, reads, writes)

    def _all_events(self):
        evs = list(self.lastw.values())
        for l in self.readers.values():
            evs.extend(l)
        for en in ENGS:
            if self.ecnt[en] > 0:
                evs.append(("es_" + en, self.ecnt[en]))
        return evs

    def barrier(self, engines=ENGS):
        evs = self._all_events()
        for e in engines:
            waits = self._filter(e, evs)
            if waits:
                self.ops[e].append((waits, None, None))
        if len(engines) == len(ENGS):
            self.lastw = {}
            self.readers = {}

    def replay(self):
        fw = self

        def run(eng, lst):
            for waits, fn, inc in lst:
                for (s, v) in waits:
                    eng.wait_ge(fw.semobj[s], v)
                if fn is None:
                    continue
                ins = fn(eng)
                if inc is not None:
                    ins.then_inc(fw.semobj[inc[0]], inc[1])

        with self.nc.Block() as block:
            @block.tensor
            def _(eng):
                run(eng, fw.ops["pe"])

            @block.scalar
            def _(eng):
                run(eng, fw.ops["act"])

            @block.vector
            def _(eng):
                run(eng, fw.ops["dve"])

            @block.gpsimd
            def _(eng):
                run(eng, fw.ops["pool"])

            @block.sync
            def _(eng):
                run(eng, fw.ops["sp"])


def _reshape(ap, shape):
    if len(shape) == 2:
        return ap
    if len(shape) == 3:
        return ap.rearrange("p (a b) -> p a b", a=shape[1])
    if len(shape) == 4:
        return ap.rearrange("p (a b c) -> p a b c", a=shape[1], b=shape[2])
    raise ValueError(shape)


class Arena:
    def __init__(self, t, ncols):
        self.t = t
        self.n = ncols
        self.off = 0

    def reset(self):
        self.off = 0

    def _take(self, n32):
        c0 = self.off
        self.off += n32
        assert self.off <= self.n, ("arena overflow", self.off, self.n)
        return self.t[:, c0:c0 + n32]

    def f32(self, shape):
        n = int(np.prod(shape[1:]))
        return _reshape(self._take(n), shape)

    def i32(self, shape):
        n = int(np.prod(shape[1:]))
        return _reshape(self._take(n).bitcast(I32), shape)

    def bf16(self, shape):
        n = int(np.prod(shape[1:]))
        n32 = (n + 1) // 2
        return _reshape(self._take(n32).bitcast(BF16)[:, 0:n], shape)


class K:
    def __init__(self, fw):
        self.fw = fw

    def mm(self, out, lhsT, rhs, start, stop, r, w, inc=True):
        self.fw.op("pe", lambda e: e.matmul(out, lhsT=lhsT, rhs=rhs, start=start, stop=stop), r, w, inc)

    def tr(self, out, in_, ident, r, w, inc=True):
        self.fw.op("pe", lambda e: e.transpose(out, in_, ident), r, w, inc)

    def act(self, out, in_, func, r, w, scale=None, bias=None, accum=None):
        kw = {}
        if scale is not None:
            kw["scale"] = scale
        if bias is not None:
            kw["bias"] = bias
        if accum is not None:
            kw["accum_out"] = accum
        self.fw.op("act", lambda e: e.activation(out=out, in_=in_, func=func, **kw), r, w)

    def tt(self, eng, out, in0, in1, op, r, w):
        self.fw.op(eng, lambda e: e.tensor_tensor(out=out, in0=in0, in1=in1, op=op), r, w)

    def ts(self, eng, out, in0, s1, op0, r, w, s2=None, op1=None):
        if s2 is None:
            self.fw.op(eng, lambda e: e.tensor_scalar(out=out, in0=in0, scalar1=s1, scalar2=None, op0=op0), r, w)
        else:
            self.fw.op(eng, lambda e: e.tensor_scalar(out=out, in0=in0, scalar1=s1, scalar2=s2, op0=op0, op1=op1), r, w)

    def stt(self, out, in0, scalar, in1, op0, op1, r, w):
        self.fw.op("dve", lambda e: e.scalar_tensor_tensor(out=out, in0=in0, scalar=scalar, in1=in1, op0=op0, op1=op1), r, w)

    def copy(self, eng, out, in_, r, w):
        if eng == "act":
            self.fw.op("act", lambda e: e.activation(out=out, in_=in_, func=AF.Copy), r, w)
        else:
            self.fw.op(eng, lambda e: e.tensor_copy(out=out, in_=in_), r, w)

    def recip(self, out, in_, r, w):
        self.fw.op("dve", lambda e: e.reciprocal(out=out, in_=in_), r, w)

    def memset(self, eng, ap, val, w):
        self.fw.op(eng, lambda e: e.memset(ap, val), (), w)

    def rsqrt(self, out, in_, scale, epsc, r, w):
        self.act(out, in_, AF.Sqrt, r, w, scale=scale, bias=epsc)
        self.recip(out, out, w, w)


def build_program():
    nc = bass.Bass("TRN2", target_bir_lowering=False)
    I = {}

    def inp(name, shape):
        I[name] = nc.dram_tensor(name, list(shape), F32, kind="ExternalInput").ap()

    inp("hT", [11, 128, 16 * 384])
    inp("vvalid", [128, 33])
    inp("xrow", [1024, 2048])
    inp("xTb", [2048, 1024])
    inp("w_a", [4, 128, 16 * 1024])
    inp("w_gate", [8, 128, 16 * 512])
    inp("w_ab", [4, 128, 8 * 512])
    inp("w_glu", [8, 128, 8 * 512])
    inp("w_out", [4, 128, 16 * 512])
    inp("w_q", [4, 128, 16 * 512])
    inp("ut", [32, 128, 16 * 512])
    inp("vv", [32, 128, 4 * 2048])
    inp("kt", [128, 16 * 128])
    inp("g1", [128, 16])
    inp("g2", [128, 16])
    inp("gqk", [128, 2])
    inp("lamv", [128, 4 * 64])
    inp("gsub", [128, 128])
    inp("ropec", [128, LP])
    inp("ropes", [128, LP])
    inp("cmat", [128, 4 * 128])
    inp("masks", [128, 4 * 512])
    inp("sa", [8, 128, 2 * 64])
    inp("sb", [8, 128, 2 * 64])
    inp("sldt", [8, 128, 1])
    inp("sS", [8, 128, 3 * 8])
    inp("sc", [8, 128, 2 * 128])
    inp("sd", [8, 128, 1])
    inp("smask", [128, 8 + 8 * 128 + 1])
    inp("iota", [128, LP])
    out_d = nc.dram_tensor("out", [1024, 2048], F32, kind="ExternalOutput").ap()
    gsc = nc.dram_tensor("gsc", [2048, 512], F32).ap()
    gsc_b = gsc.bitcast(BF16)

    with contextlib.ExitStack() as st:
        fw = FW(nc, st)
        k = K(fw)
        ARENA_COLS = 53200
        arena_t = st.enter_context(nc.sbuf_tensor("arena", [128, ARENA_COLS], F32))
        ar = Arena(arena_t, ARENA_COLS)
        ps = [st.enter_context(nc.psum_tensor("bank%d" % i, [128, 512], F32)) for i in range(8)]
        psk = ["ps%d" % i for i in range(8)]

        def psbf(i):
            return ps[i][:, :].bitcast(BF16)

        cm = ar.bf16([128, 4, 128])
        ident, perm, bones, ones = cm[:, 0, :], cm[:, 1, :], cm[:, 2, :], cm[:, 3, :]
        fw.dma("pool", cm, I["cmat"].rearrange("p (a b) -> p a b", a=4), writes=["cm"], slot="s_cm")
        msk = ar.bf16([128, 4, 512])
        fw.dma("pool", msk, I["masks"].rearrange("p (a b) -> p a b", a=4), writes=["msk"], slot="s_msk")
        g1 = ar.f32([128, 16]); fw.dma("sp", g1, I["g1"], writes=["g1"], slot="s_g1")
        gqk = ar.f32([128, 2]); fw.dma("sp", gqk, I["gqk"], writes=["gqk"], slot="s_gqk")
        lamv = ar.f32([128, 4, 64])
        fw.dma("sp", lamv, I["lamv"].rearrange("p (a b) -> p a b", a=4), writes=["lamv"], slot="s_lamv")
        gsub = ar.f32([128, 128]); fw.dma("sp", gsub, I["gsub"], writes=["gsub"], slot="s_gsub")
        vval = ar.f32([128, 33]); fw.dma("sp", vval, I["vvalid"], writes=["vval"], slot="s_vval")
        smk = ar.f32([128, 8 + 1024 + 1]); fw.dma("sp", smk, I["smask"], writes=["smk"], slot="s_smk")
        rowmask = smk[:, 0:8]
        colmask = smk[:, 8:8 + 1024].rearrange("p (a b) -> p a b", a=8)
        sgn = smk[:, 1032:1033]
        iota = ar.f32([128, LP]); fw.dma("sp", iota, I["iota"], writes=["iota"], slot="s_iota")
        epsc = ar.f32([128, 1])
        k.memset("dve", epsc, EPS, ["epsc"])
        onec = ar.f32([128, 1])
        k.memset("dve", onec, 1.0, ["onec"])
        magc = ar.f32([128, 2])
        k.memset("dve", magc[:, 0:1], 12582912.0, ["magc"])
        k.memset("dve", magc[:, 1:2], -12582912.0, ["magc"])
        k.ts("dve", gsub, gsub, 1.0 - LAM_INIT, ALU.mult, ["gsub"], ["gsub"])
        lp = ar.f32([128, 2, 64]); lsum = ar.f32([128, 2]); lexp = ar.f32([128, 2]); neglam = ar.f32([128, 1])
        k.tt("dve", lp[:, 0, :], lamv[:, 0, :], lamv[:, 1, :], ALU.mult, ["lamv"], ["lp"])
        k.tt("dve", lp[:, 1, :], lamv[:, 2, :], lamv[:, 3, :], ALU.mult, ["lamv"], ["lp"])
        fw.op("dve", lambda e: e.tensor_reduce(out=lsum, in_=lp, axis=AX.X, op=ALU.add), ["lp"], ["lsum"])
        k.act(lexp, lsum, AF.Exp, ["lsum"], ["lexp"])
        k.tt("dve", neglam, lexp[:, 1:2], lexp[:, 0:1], ALU.subtract, ["lexp"], ["neglam"])
        k.ts("dve", neglam, neglam, -LAM_INIT, ALU.add, ["neglam"], ["neglam"])
        rstd_all = ar.f32([128, LP])
        mark_pass = ar.off
        hT = I["hT"]
        WIN0 = LP - 1024
        MAGIC = 12582912.0

        for p in range(4):
            fw.barrier()
            ar.off = mark_pass
            Wa = ar.bf16([128, 16, 1024])
            fw.dma("pool", Wa, I["w_a"][p].rearrange("p (k f) -> p k f", k=16), writes=["Wa"], slot="s_wa")
            for kk in range(16):
                k.ts("dve", Wa[:, kk, :], Wa[:, kk, :], g1[:, kk:kk + 1], ALU.mult, ["Wa", "g1"], ["Wa"])
            QT = ar.bf16([128, 2, 1152])
            KT = ar.bf16([128, 2, LP])
            UT = ar.bf16([128, 2, LP])
            Vtok = ar.bf16([128, 33, 2, 130])
            for hl in range(2):
                k.copy("dve", Vtok[:, :, hl, 128:129], vval.unsqueeze(2), ["vval"], ["Vtok"])
            mark_a = ar.off
            xbs = [ar.bf16([128, 16, 384]) for _ in range(2)]
            xsq = ar.bf16([128, 16, 384])
            rcs = [ar.f32([128, 384]) for _ in range(2)]
            rss = [ar.f32([128, 384]) for _ in range(2)]
            qs_ = [ar.f32([128, 384]) for _ in range(2)]; sqb_ = [ar.bf16([128, 384]) for _ in range(2)]
            rq_ = [ar.f32([128, 384]) for _ in range(2)]; qn_ = [ar.bf16([128, 384]) for _ in range(2)]
            t1_ = [ar.f32([128, 384]) for _ in range(2)]; t2_ = [ar.f32([128, 384]) for _ in range(2)]
            vsb_ = [ar.bf16([128, 384]) for _ in range(2)]
            fc = 0
            pbc = 0
            for ci in range(11):
                b = ci % 2
                col0 = ci * 384
                cols = slice(col0, col0 + 384)
                xb = xbs[b]
                xbk = "xb%d" % b
                fw.dma("pool", xb, hT[ci].rearrange("p (k t) -> p k t", k=16), writes=[xbk], slot=xbk)
                fw.dma("sp", rcs[b], I["ropec"][:, cols], writes=["rc%d" % b], slot="rc%d" % b)
                fw.dma("sp", rss[b], I["ropes"][:, cols], writes=["rs%d" % b], slot="rs%d" % b)
                rstd = rstd_all[:, cols]
                if p == 0:
                    k.act(xsq, xb, AF.Square, [xbk], ["xsq"])
                    for kk in range(16):
                        k.mm(ps[0][:, 0:384], ones, xsq[:, kk, :], kk == 0, kk == 15, ["cm", "xsq"], ["ps0"], inc=(kk == 15))
                    k.rsqrt(rstd, ps[0][:, 0:384], 1.0 / 2048, epsc, ["ps0", "epsc"], ["rstd"])
                fts = [("k", 0, 2), ("k", 1, 3), ("v", 0, 4), ("v", 1, 5), ("u", 0, 6), ("u", 1, 7)]
                if ci >= 8:
                    fts = [("q", 0, 0), ("q", 1, 1)] + fts
                for kind, hl, f in fts:
                    pb = 1 + pbc % 4
                    pbc += 1
                    pbk = psk[pb]
                    for kk in range(16):
                        k.mm(ps[pb][:, 0:384], Wa[:, kk, f * 128:(f + 1) * 128], xb[:, kk, :], kk == 0, kk == 15,
                             ["Wa", xbk], [pbk], inc=(kk == 15))
                    if kind in ("q", "k"):
                        gi = 0 if kind == "q" else 1
                        z = fc % 2
                        fc += 1
                        qs, sqb, rq, qn, t1, t2 = qs_[z], sqb_[z], rq_[z], qn_[z], t1_[z], t2_[z]
                        zs = str(z)
                        pn = 5 + z
                        k.tt("dve", qs, ps[pb][:, 0:384], rstd, ALU.mult, [pbk, "rstd"], ["qs" + zs])
                        k.act(sqb, qs, AF.Square, ["qs" + zs], ["sqb" + zs])
                        k.mm(ps[pn][:, 0:384], bones, sqb, True, True, ["cm", "sqb" + zs], [psk[pn]])
                        k.rsqrt(rq, ps[pn][:, 0:384], 1.0 / 64, epsc, [psk[pn], "epsc"], ["rq" + zs])
                        k.stt(qn, qs, gqk[:, gi:gi + 1], rq, ALU.mult, ALU.mult, ["qs" + zs, "gqk", "rq" + zs], ["qn" + zs])
                        k.mm(ps[pn][:, 0:384], perm, qn, True, True, ["cm", "qn" + zs], [psk[pn]])
                        k.tt("dve", t1, qn, rcs[b], ALU.mult, ["qn" + zs, "rc%d" % b], ["t1" + zs])
                        k.tt("dve", t2, ps[pn][:, 0:384], rss[b], ALU.mult, [psk[pn], "rs%d" % b], ["t2" + zs])
                        if kind == "q":
                            dst = QT[:, hl, (ci - 8) * 384:(ci - 8) * 384 + 384]
                            dk = "QT"
                        else:
                            dst = KT[:, hl, cols]
                            dk = "KT"
                        k.tt("dve", dst, t1, t2, ALU.add, ["t1" + zs, "t2" + zs], [dk])
                    elif kind == "v":
                        z = fc % 2
                        fc += 1
                        vsb = vsb_[z]
                        k.tt("dve", vsb, ps[pb][:, 0:384], rstd, ALU.mult, [pbk, "rstd"], ["vsb%d" % z])
                        p7 = psbf(7)
                        for j in range(3):
                            k.tr(p7[:, j * 128:(j + 1) * 128], vsb[:, j * 128:(j + 1) * 128], ident, ["vsb%d" % z, "cm"], ["ps7"])
                        k.copy("act", Vtok[:, ci * 3:ci * 3 + 3, hl, 0:128],
                               p7[:, 0:384].rearrange("p (a b) -> p a b", a=3), ["ps7"], ["Vtok"])
                    else:
                        k.tt("dve", UT[:, hl, cols], ps[pb][:, 0:384], rstd, ALU.mult, [pbk, "rstd"], ["UT"])

            fw.barrier()
            ar.off = mark_a
            Pb = [ar.bf16([128, 512]) for _ in range(2)]
            Asb = [ar.f32([128, 4, 128]) for _ in range(2)]
            rl = ar.f32([128, 1])
            attf = ar.f32([128, 4, 128])
            junk = ar.f32([128, 128])
            ss4 = ar.f32([128, 4])
            attn = ar.bf16([128, 4, 128])
            attT = [ar.bf16([128, 512]) for _ in range(2)]
            ai = 0
            sa = ar.f32([128, 2, 64]); sbb = ar.f32([128, 2, 64]); sldt = ar.f32([128, 1])
            sS = ar.f32([128, 3, 8]); scc = ar.f32([128, 2, 128]); sd_ = [ar.f32([128, 1]) for _ in range(2)]
            dtc = ar.f32([128, 1])
            ard = ar.f32([128, 64]); aid = ar.f32([128, 64]); rr = ar.f32([128, 64])
            yy = ar.f32([128, 64]); kf = ar.f32([128, 64]); ff = ar.f32([128, 64])
            sn = ar.f32([128, 64]); hh = ar.f32([128, 64]); cs = ar.f32([128, 64])
            abr = ar.f32([128, 64]); abi = ar.f32([128, 64]); den = ar.f32([128, 64]); tq = ar.f32([128, 64])
            mr = ar.f32([128, 64]); mi = ar.f32([128, 64]); Bre = ar.f32([128, 64]); Bim = ar.f32([128, 64])
            Bpad_ = [ar.bf16([128, 8, 128]) for _ in range(2)]; Bswp_ = [ar.bf16([128, 8, 128]) for _ in range(2)]
            CAs = ar.f32([128, 128]); CBs = ar.f32([128, 128])
            CpA_ = [ar.bf16([128, 8, 128]) for _ in range(2)]; CpB_ = [ar.bf16([128, 8, 128]) for _ in range(2)]
            dtS = ar.f32([128, 8]); rS_ = [ar.f32([128, 8]) for _ in range(2)]; thS_ = [ar.f32([128, 8]) for _ in range(2)]
            carr = ar.f32([128, 8])
            TC = 512
            tys = [ar.f32([128, TC]) for _ in range(2)]; tkf = ar.f32([128, TC])
            COS = [ar.f32([128, TC]) for _ in range(2)]
            SIN = [ar.f32([128, TC]) for _ in range(2)]
            th_ = ar.f32([128, TC])
            w1s = [ar.f32([128, TC]) for _ in range(2)]; w2s = [ar.f32([128, TC]) for _ in range(2)]; zzs = [ar.f32([128, TC]) for _ in range(2)]
            u1 = [ar.bf16([128, TC]) for _ in range(2)]
            u2 = [ar.bf16([128, TC]) for _ in range(2)]
            ysf = ar.f32([128, TC])
            ysb = [ar.bf16([128, TC]) for _ in range(2)]
            W = ["ssmtmp"]
            ti = 0
            yic = [0]

            def ssm_setup(tl):
                Bpad, Bswp, CpA, CpB, rS, thS, sd = Bpad_[tl], Bswp_[tl], CpA_[tl], CpB_[tl], rS_[tl], thS_[tl], sd_[tl]
                kB, kC, kr, kth, ksd = 'Bpad%d' % tl, 'Cpad%d' % tl, 'rS%d' % tl, 'thS%d' % tl, 'sd%d' % tl
                T = 2 * p + tl
                fw.dma("sp", sa, I["sa"][T].rearrange("p (a b) -> p a b", a=2), writes=["sa"], slot="s_sa")
                fw.dma("sp", sbb, I["sb"][T].rearrange("p (a b) -> p a b", a=2), writes=["sbb"], slot="s_sb")
                fw.dma("sp", sldt, I["sldt"][T], writes=["sldt"], slot="s_sldt")
                fw.dma("sp", sS, I["sS"][T].rearrange("p (a b) -> p a b", a=3), writes=["sS"], slot="s_sS")
                fw.dma("sp", scc, I["sc"][T].rearrange("p (a b) -> p a b", a=2), writes=["scc"], slot="s_sc")
                fw.dma("sp", sd, I["sd"][T], writes=[ksd], slot="s_sd")
                k.act(dtc, sldt, AF.Exp, ["sldt"], W)
                k.ts("dve", ard, sa[:, 0, :], dtc[:, 0:1], ALU.mult, ["sa"] + W, W)
                k.ts("dve", aid, sa[:, 1, :], dtc[:, 0:1], ALU.mult, ["sa"] + W, W)
                k.act(rr, ard, AF.Exp, W, W)
                k.ts("dve", yy, aid, 1.0 / TWO_PI, ALU.mult, W, W)
                k.ts("dve", kf, yy, MAGIC, ALU.add, W, W)
                k.ts("dve", kf, kf, -MAGIC, ALU.add, W, W)
                k.tt("dve", ff, yy, kf, ALU.subtract, W, W)
                k.act(sn, ff, AF.Sin, W, W, scale=TWO_PI)
                k.act(hh, ff, AF.Sin, W, W, scale=math.pi)
                k.tt("dve", hh, hh, hh, ALU.mult, W, W)
                k.ts("dve", cs, hh, -2.0, ALU.mult, W, W, s2=1.0, op1=ALU.add)
                k.tt("dve", abr, rr, cs, ALU.mult, W, W)
                k.ts("dve", abr, abr, -1.0, ALU.add, W, W)
                k.tt("dve", abi, rr, sn, ALU.mult, W, W)
                k.tt("dve", den, sa[:, 0, :], sa[:, 0, :], ALU.mult, ["sa"] + W, W)
                k.tt("dve", tq, sa[:, 1, :], sa[:, 1, :], ALU.mult, ["sa"] + W, W)
                k.tt("dve", den, den, tq, ALU.add, W, W)
                k.recip(den, den, W, W)
                k.tt("dve", mr, abr, sa[:, 0, :], ALU.mult, ["sa"] + W, W)
                k.tt("dve", tq, abi, sa[:, 1, :], ALU.mult, ["sa"] + W, W)
                k.tt("dve", mr, mr, tq, ALU.add, W, W)
                k.tt("dve", mr, mr, den, ALU.mult, W, W)
                k.tt("dve", mi, abi, sa[:, 0, :], ALU.mult, ["sa"] + W, W)
                k.tt("dve", tq, abr, sa[:, 1, :], ALU.mult, ["sa"] + W, W)
                k.tt("dve", mi, mi, tq, ALU.subtract, W, W)
                k.tt("dve", mi, mi, den, ALU.mult, W, W)
                k.tt("dve", Bre, mr, sbb[:, 0, :], ALU.mult, ["sbb"] + W, W)
                k.tt("dve", tq, mi, sbb[:, 1, :], ALU.mult, ["sbb"] + W, W)
                k.tt("dve", Bre, Bre, tq, ALU.subtract, W, W)
                k.tt("dve", Bim, mr, sbb[:, 1, :], ALU.mult, ["sbb"] + W, W)
                k.tt("dve", tq, mi, sbb[:, 0, :], ALU.mult, ["sbb"] + W, W)
                k.tt("dve", Bim, Bim, tq, ALU.add, W, W)
                rmb = rowmask.unsqueeze(2).to_broadcast([128, 8, 64])
                breb = Bre.unsqueeze(1).to_broadcast([128, 8, 64])
                bimb = Bim.unsqueeze(1).to_broadcast([128, 8, 64])
                k.tt("dve", Bpad[:, :, 0:64], breb, rmb, ALU.mult, ["smk"] + W, [kB])
                k.tt("dve", Bpad[:, :, 64:128], bimb, rmb, ALU.mult, ["smk"] + W, [kB])
                k.tt("dve", Bswp[:, :, 0:64], bimb, rmb, ALU.mult, ["smk"] + W, [kB])
                k.ts("dve", tq, Bre, -1.0, ALU.mult, W, W)
                k.tt("dve", Bswp[:, :, 64:128], tq.unsqueeze(1).to_broadcast([128, 8, 64]), rmb, ALU.mult, ["smk"] + W, [kB])
                k.ts("dve", CAs, scc[:, 0, :], sgn, ALU.mult, ["scc", "smk"], W)
                k.ts("dve", CBs, scc[:, 1, :], -1.0, ALU.mult, ["scc"], W)
                k.tt("dve", CpA, CAs.unsqueeze(1).to_broadcast([128, 8, 128]), colmask, ALU.mult, ["smk"] + W, [kC])
                k.tt("dve", CpB, CBs.unsqueeze(1).to_broadcast([128, 8, 128]), colmask, ALU.mult, ["smk"] + W, [kC])
                k.act(dtS, sS[:, 2, :], AF.Exp, ["sS"], ["dtS"])
                k.tt("dve", rS, sS[:, 0, :], dtS, ALU.mult, ["sS", "dtS"], [kr])
                k.act(rS, rS, AF.Exp, [kr], [kr])
                k.tt("dve", thS, sS[:, 1, :], dtS, ALU.mult, ["sS", "dtS"], [kth])
                k.ts("dve", thS, thS, 1.0 / TWO_PI, ALU.mult, [kth], [kth])

            def ssm_loop(tl):
                Bpad, Bswp, CpA, CpB, rS, thS, sd = Bpad_[tl], Bswp_[tl], CpA_[tl], CpB_[tl], rS_[tl], thS_[tl], sd_[tl]
                kB, kC, kr, kth, ksd = 'Bpad%d' % tl, 'Cpad%d' % tl, 'rS%d' % tl, 'thS%d' % tl, 'sd%d' % tl
                T = 2 * p + tl
                sunits = [(tc, g) for tc in range(9) for g in range(8)]

                def dims(tc):
                    n = TC if tc < 8 else LP - 8 * TC
                    return n, tc * TC

                def emit_tab1a(ui):
                    tc, g = sunits[ui]
                    n, p0_ = dims(tc)
                    cols = slice(p0_, p0_ + n)
                    k.act(tkf[:, 0:n], iota[:, cols], AF.Identity, ["iota", kth, "magc"], ["tkf"], scale=thS[:, g:g + 1], bias=magc[:, 0:1])
                    k.act(tkf[:, 0:n], tkf[:, 0:n], AF.Identity, ["tkf", "magc"], ["tkf"], bias=magc[:, 1:2])

                def emit_tab1b(ui):
                    tc, g = sunits[ui]
                    n, p0_ = dims(tc)
                    cols = slice(p0_, p0_ + n)
                    tyb = tys[ui % 2]
                    k.stt(tyb[:, 0:n], iota[:, cols], thS[:, g:g + 1], tkf[:, 0:n], ALU.mult, ALU.subtract, ["iota", kth, "tkf"], ["ty%d" % (ui % 2)])

                def emit_tab2(ui):
                    tc, g = sunits[ui]
                    n, p0_ = dims(tc)
                    tb = ui % 2
                    tk = "tab%d" % tb
                    tyb = tys[tb]
                    k.act(SIN[tb][:, 0:n], tyb[:, 0:n], AF.Sin, ["ty%d" % tb], [tk], scale=TWO_PI)
                    k.act(th_[:, 0:n], tyb[:, 0:n], AF.Sin, ["ty%d" % tb], ["th_"], scale=math.pi)
                    k.act(th_[:, 0:n], th_[:, 0:n], AF.Square, ["th_"], ["th_"])
                    k.act(COS[tb][:, 0:n], th_[:, 0:n], AF.Identity, ["th_", "onec"], [tk], scale=-2.0, bias=onec)

                NS = len(sunits)
                emit_tab1a(0); emit_tab1b(0); emit_tab2(0)
                emit_tab1a(1); emit_tab1b(1)
                for ui in range(NS):
                    tc, g = sunits[ui]
                    n, p0_ = dims(tc)
                    cols = slice(p0_, p0_ + n)
                    outp = tc >= 6
                    tb = ui % 2
                    tk = "tab%d" % tb
                    w1, w2, zz = w1s[tb], w2s[tb], zzs[tb]
                    wk1, wk2, zk = "w1%d" % tb, "w2%d" % tb, "zz%d" % tb
                    b1, b2 = (0, 1) if tb == 0 else (3, 4)
                    k.mm(ps[b1][:, 0:n], Bpad[:, g, :], UT[:, tl, cols], True, True, [kB, "UT"], [psk[b1]])
                    k.mm(ps[b2][:, 0:n], Bswp[:, g, :], UT[:, tl, cols], True, True, [kB, "UT"], [psk[b2]])
                    if ui + 1 < NS:
                        emit_tab2(ui + 1)
                    if ui + 2 < NS:
                        emit_tab1a(ui + 2)
                    k.tt("dve", w1[:, 0:n], ps[b1][:, 0:n], COS[tb][:, 0:n], ALU.mult, [psk[b1], tk], [wk1])
                    k.tt("dve", w2[:, 0:n], ps[b2][:, 0:n], SIN[tb][:, 0:n], ALU.mult, [psk[b2], tk], [wk2])
                    k.tt("dve", w1[:, 0:n], w1[:, 0:n], w2[:, 0:n], ALU.add, [wk1, wk2], [wk1])
                    rsb = rS[:, g:g + 1].to_broadcast([128, n])
                    init = 0.0 if tc == 0 else carr[:, g:g + 1]

                    def scan(e, rsb=rsb, init=init, n=n, zz=zz, w1=w1):
                        return e.tensor_tensor_scan(out=zz[:, 0:n], data0=rsb, data1=w1[:, 0:n], initial=init,
                                                    op0=ALU.mult, op1=ALU.add)
                    fw.op("dve", scan, [kr, wk1, "carr"], [zk])
                    k.copy("dve", carr[:, g:g + 1], zz[:, n - 1:n], [zk], ["carr"])
                    if ui + 2 < NS:
                        emit_tab1b(ui + 2)
                    if outp:
                        ub = g % 2
                        k.tt("dve", u1[ub][:, 0:n], zz[:, 0:n], COS[tb][:, 0:n], ALU.mult, [zk, tk], ["u1%d" % ub])
                        k.tt("dve", u2[ub][:, 0:n], zz[:, 0:n], SIN[tb][:, 0:n], ALU.mult, [zk, tk], ["u2%d" % ub])
                        k.mm(ps[2][:, 0:n], CpA[:, g, :], u1[ub][:, 0:n], g == 0, False, [kC, "u1%d" % ub], ["ps2"])
                        k.mm(ps[2][:, 0:n], CpB[:, g, :], u2[ub][:, 0:n], False, g == 7, [kC, "u2%d" % ub], ["ps2"])
                    if outp and g == 7:
                        k.stt(ysf[:, 0:n], UT[:, tl, cols], sd[:, 0:1], ps[2][:, 0:n], ALU.mult, ALU.add,
                              ["UT", ksd, "ps2"], ["ysf"])
                        yb = ysb[yic[0] % 2]
                        yk = "ysb%d" % (yic[0] % 2)
                        yic[0] += 1
                        k.act(yb[:, 0:n], ysf[:, 0:n], AF.Gelu, ["ysf"], [yk])
                        lo = max(p0_, WIN0)
                        r0 = T * 256 + 128
                        fw.dma("sp", gsc_b[r0:r0 + 128, lo - WIN0:p0_ + n - WIN0], yb[:, lo - p0_:n],
                               reads=[yk], writes=["gsc"], slot="gsc")

            ssm_setup(0)
            ssm_setup(1)
            for hl in range(2):
                for qc in range(2):
                    kd = 25 + 4 * qc
                    nkt = kd + 4
                    for sub in range(2):
                        pr = slice(sub * 64, sub * 64 + 64)
                        def emit_s(kt_):
                            sb_ = kt_ % 2
                            k.mm(ps[sb_][:, :], KT[pr, hl, kt_ * 128:kt_ * 128 + 128],
                                 QT[pr, hl, 128 + qc * 512:128 + qc * 512 + 512], True, True, ["KT", "QT"], [psk[sb_]])
                            k.act(Pb[sb_], ps[sb_][:, :], AF.Exp, [psk[sb_]], ["P%d" % sb_], scale=0.125)

                        emit_s(0)
                        for kt_ in range(nkt):
                            sb_ = kt_ % 2
                            if kt_ + 1 < nkt:
                                emit_s(kt_ + 1)
                            P = Pb[sb_]
                            pk = "P%d" % sb_
                            i = kt_ - kd
                            if i >= 0:
                                k.tt("dve", P, P, msk[:, i, :], ALU.mult, [pk, "msk"], [pk])
                            for j in range(4):
                                if kt_ <= kd + j:
                                    k.mm(ps[2 + j][:, 0:129], P[:, j * 128:(j + 1) * 128], Vtok[:, kt_, hl, 0:129],
                                         kt_ == 0, kt_ == kd + j, [pk, "Vtok"], [psk[2 + j]])
                        for j in range(4):
                            k.recip(rl, ps[2 + j][:, 128:129], [psk[2 + j]], ["rl"])
                            k.ts("dve", Asb[sub][:, j, :], ps[2 + j][:, 0:128], rl[:, 0:1], ALU.mult,
                                 [psk[2 + j], "rl"], ["A%d" % sub])
                    for j in range(4):
                        k.stt(attf[:, j, :], Asb[1][:, j, :], neglam[:, 0:1], Asb[0][:, j, :], ALU.mult, ALU.add,
                              ["A0", "A1", "neglam"], ["attf"])
                        k.act(junk, attf[:, j, :], AF.Square, ["attf"], ["junk", "ss4"], accum=ss4[:, j:j + 1])
                    k.rsqrt(ss4, ss4, 1.0 / 128, epsc, ["ss4", "epsc"], ["ss4"])
                    p6 = psbf(6)
                    for j in range(4):
                        k.stt(attn[:, j, :], attf[:, j, :], ss4[:, j:j + 1], gsub, ALU.mult, ALU.mult,
                              ["attf", "ss4", "gsub"], ["attn"])
                        k.tr(p6[:, j * 128:(j + 1) * 128], attn[:, j, :], ident, ["attn", "cm"], ["ps6"])
                    aT = attT[ai % 2]
                    ak = "attT%d" % (ai % 2)
                    ai += 1
                    k.copy("act", aT, p6[:, 0:512], ["ps6"], [ak])
                    r0 = (2 * p + hl) * 256
                    fw.dma("sp", gsc_b[r0:r0 + 128, qc * 512:qc * 512 + 512], aT, reads=[ak], writes=["gsc"], slot="gsc")

            ssm_loop(0)
            ssm_loop(1)
        if DEBUG:
            dbg = nc.dram_tensor("dbg_gsc", [2048, 512], F32, kind="ExternalOutput").ap()
            fw.dma("sp", dbg, gsc, reads=["gsc"], writes=["dbg"], slot="dbg")
        fw.barrier()

        ar.reset()
        cm = ar.bf16([128, 4, 128])
        ident = cm[:, 0, :]
        ar.off = mark_pb = ar.off
        zeroT = cm[:, 1, :]
        k.memset("dve", zeroT, 0.0, ["cm"])
        g1 = ar.f32([128, 16]); fw.dma("sp", g1, I["g1"], writes=["g1"], slot="s_g1")
        g2 = ar.f32([128, 16]); fw.dma("sp", g2, I["g2"], writes=["g2"], slot="s_g2")
        epsc = ar.f32([128, 1]); k.memset("dve", epsc, EPS, ["epsc"])
        KTb = ar.bf16([128, 16, 128])
        fw.dma("pool", KTb, I["kt"].rearrange("p (a b) -> p a b", a=16), writes=["KTb"], slot="s_kt")
        res = ar.f32([128, 4, 2048])
        T1 = ar.bf16([128, 16, 512])
        T2 = ar.bf16([128, 16, 512])
        big = ar._take(8192)
        mix = _reshape(big[:, 0:4096].bitcast(BF16), [128, 4, 2048])
        sfl = big
        s4 = big.rearrange("p (t f n) -> p t f n", t=4, f=16)
        Rw = [ar.bf16([128, 16, 512]) for _ in range(2)]
        Rv = [ar.bf16([128, 16, 512]) for _ in range(2)]
        scr = ar.bf16([128, 2048])
        ssq = ar.f32([128, 4]); r1 = ar.f32([128, 4]); r2 = ar.f32([128, 4])
        sab = ar.bf16([128, 512]); sbb2 = ar.bf16([128, 512]); sgb = ar.bf16([128, 512])
        ta = ar.f32([128, 512]); tb2 = ar.f32([128, 512])
        Dall = ar.f32([128, 8, 512])
        Dflat = Dall.rearrange("p h e -> p (h e)")
        cand = Dflat[:, 0:2048].rearrange("p (h e) -> p h e", h=8)
        tmp256 = Dflat[:, 2048:2304]
        tmp128 = Dflat[:, 2304:2432]
        v16 = Dflat[:, 2432:2688].rearrange("p (a b) -> p a b", a=16)
        top = ar.f32([128, 8, 16])
        tau = ar.f32([128, 4, 8]); nb = ar.f32([128, 4, 8])
        negm = ar.f32([128, 8]); Zs = ar.f32([128, 8]); lnZ = ar.f32([128, 8])
        junk16 = ar.f32([128, 16])
        gas = [ar.bf16([128, 512]) for _ in range(2)]
        wb = ar.bf16([128, 512])
        EX = ar.bf16([128, 8, 512])
        wTs = [ar.bf16([128, 4, 128]) for _ in range(2)]


        for hf in range(2):
            tsl = slice(hf * 512, hf * 512 + 512)
            fw.dma("sp", res, I["xrow"][tsl, :].rearrange("(t p) d -> p t d", p=128), writes=["res"], slot="res")
            fw.dma("pool", T1, I["xTb"][:, tsl].rearrange("(k p) t -> p k t", p=128), writes=["T1"], slot="T1")
            gv = gsc_b[:, tsl].rearrange("(r a p) t -> a p r t", a=2, p=128)
            for a in range(2):
                fw.dma("sp", T2[:, a * 8:a * 8 + 8, :], gv[a], reads=["gsc"], writes=["T2"], slot="T2")
            for tt in range(4):
                k.act(scr, res[:, tt, :], AF.Square, ["res"], ["scr", "ssq"], accum=ssq[:, tt:tt + 1])
            k.rsqrt(r1, ssq, 1.0 / 2048, epsc, ["ssq", "epsc"], ["r1"])
            k.tt("dve", T1, T1, g1.unsqueeze(2).to_broadcast([128, 16, 512]), ALU.mult, ["T1", "g1"], ["T1"])
            for n4 in range(4):
                c0 = n4 * 512
                fw.dma("pool", Rw[0], I["w_gate"][n4].rearrange("p (k f) -> p k f", k=16), writes=["Rw0"], slot="Rw0")
                fw.dma("pool", Rw[1], I["w_gate"][4 + n4].rearrange("p (k f) -> p k f", k=16), writes=["Rw1"], slot="Rw1")
                fw.dma("pool", Rv[0][:, 0:8, :], I["w_ab"][n4].rearrange("p (k f) -> p k f", k=8), writes=["Rv0"], slot="Rv0")
                fw.dma("pool", Rv[1][:, 0:8, :], I["w_glu"][n4].rearrange("p (k f) -> p k f", k=8), writes=["Rv1"], slot="Rv1")
                fw.dma("pool", Rv[1][:, 8:16, :], I["w_glu"][4 + n4].rearrange("p (k f) -> p k f", k=8), writes=["Rv1"], slot="Rv1")
                for tt in range(4):
                    tk_ = slice(tt * 128, tt * 128 + 128)
                    for kk in range(16):
                        k.mm(ps[0][:, :], T1[:, kk, tk_], Rw[0][:, kk, :], kk == 0, kk == 15, ["T1", "Rw0"], ["ps0"], inc=(kk == 15))
                    for kk in range(16):
                        k.mm(ps[1][:, :], T1[:, kk, tk_], Rw[1][:, kk, :], kk == 0, kk == 15, ["T1", "Rw1"], ["ps1"], inc=(kk == 15))
                    for kk in range(8):
                        k.mm(ps[2][:, :], T2[:, kk, tk_], Rv[0][:, kk, :], kk == 0, kk == 7, ["T2", "Rv0"], ["ps2"], inc=(kk == 7))
                    for kk in range(8):
                        k.mm(ps[3][:, :], T2[:, 8 + kk, tk_], Rv[1][:, kk, :], kk == 0, kk == 7, ["T2", "Rv1"], ["ps3"], inc=(kk == 7))
                    for kk in range(8):
                        k.mm(ps[4][:, :], T2[:, 8 + kk, tk_], Rv[1][:, 8 + kk, :], kk == 0, kk == 7, ["T2", "Rv1"], ["ps4"], inc=(kk == 7))
                    k.act(sab, ps[0][:, :], AF.Sigmoid, ["ps0", "r1"], ["sab"], scale=r1[:, tt:tt + 1])
                    k.act(sbb2, ps[1][:, :], AF.Sigmoid, ["ps1", "r1"], ["sbb2"], scale=r1[:, tt:tt + 1])
                    k.act(sgb, ps[4][:, :], AF.Sigmoid, ["ps4"], ["sgb"])
                    k.tt("dve", ta, ps[2][:, :], sab, ALU.mult, ["ps2", "sab"], ["ta"])
                    k.tt("dve", tb2, ps[3][:, :], sgb, ALU.mult, ["ps3", "sgb"], ["tb2"])
                    k.tt("pool", tb2, tb2, sbb2, ALU.mult, ["tb2", "sbb2"], ["tb2"])
                    k.tt("pool", mix[:, tt, c0:c0 + 512], ta, tb2, ALU.add, ["ta", "tb2"], ["big"])
            p7 = psbf(7)
            for tt in range(4):
                for half8 in range(2):
                    for kk in range(8):
                        kf_ = half8 * 8 + kk
                        k.tr(p7[:, kk * 128:(kk + 1) * 128], mix[:, tt, kf_ * 128:(kf_ + 1) * 128], ident, ["big", "cm"], ["ps7"])
                    k.copy("act", T2[:, half8 * 8:half8 * 8 + 8, tt * 128:(tt + 1) * 128],
                           p7.rearrange("p (a b) -> p a b", a=8), ["ps7"], ["T2"])
            for n4 in range(4):
                c0 = n4 * 512
                rb = n4 % 2
                fw.dma("pool", Rw[rb], I["w_out"][n4].rearrange("p (k f) -> p k f", k=16), writes=["Rw%d" % rb], slot="Rw%d" % rb)
                for tt in range(4):
                    tk_ = slice(tt * 128, tt * 128 + 128)
                    pb_ = tt % 2
                    for kk in range(16):
                        k.mm(ps[pb_][:, :], T2[:, kk, tk_], Rw[rb][:, kk, :], kk == 0, kk == 15, ["T2", "Rw%d" % rb], [psk[pb_]], inc=(kk == 15))
                    k.tt("dve", res[:, tt, c0:c0 + 512], ps[pb_][:, :], res[:, tt, c0:c0 + 512], ALU.add, [psk[pb_], "res"], ["res"])
            for tt in range(4):
                k.act(scr, res[:, tt, :], AF.Square, ["res"], ["scr", "ssq"], accum=ssq[:, tt:tt + 1])
            k.rsqrt(r2, ssq, 1.0 / 2048, epsc, ["ssq", "epsc"], ["r2"])
            for tt in range(4):
                k.copy("act", scr, res[:, tt, :], ["res"], ["scr"])
                for half8 in range(2):
                    for kk in range(8):
                        kf_ = half8 * 8 + kk
                        k.tr(p7[:, kk * 128:(kk + 1) * 128], scr[:, kf_ * 128:(kf_ + 1) * 128], ident, ["scr", "cm"], ["ps7"])
                    k.copy("dve", T1[:, half8 * 8:half8 * 8 + 8, tt * 128:(tt + 1) * 128],
                           p7.rearrange("p (a b) -> p a b", a=8), ["ps7"], ["T1"])
            k.tt("dve", T1, T1, g2.unsqueeze(2).to_broadcast([128, 16, 512]), ALU.mult, ["T1", "g2"], ["T1"])
            for wc in range(4):
                rb = wc % 2
                fw.dma("pool", Rw[rb], I["w_q"][wc].rearrange("p (k f) -> p k f", k=16), writes=["Rw%d" % rb], slot="Rw%d" % rb)
                for f4 in range(4):
                    ft = wc * 4 + f4
                    pb_ = f4 % 2
                    for kk in range(16):
                        k.mm(ps[pb_][:, :], Rw[rb][:, kk, f4 * 128:(f4 + 1) * 128], T1[:, kk, :], kk == 0, kk == 15, ["T1", "Rw%d" % rb], [psk[pb_]], inc=(kk == 15))
                    k.copy("act", T2[:, ft, :], ps[pb_][:, :], [psk[pb_]], ["T2"])
            for tt in range(4):
                tk_ = slice(tt * 128, tt * 128 + 128)
                for ft in range(16):
                    pb_ = 2 + ft // 4
                    k.mm(ps[pb_][:, (ft % 4) * 128:(ft % 4) * 128 + 128], T2[:, ft, tk_], KTb[:, ft, :], True, True, ["T2", "KTb"], [psk[pb_]])
                for f4 in range(4):
                    k.ts("dve", sfl[:, tt * 2048 + f4 * 512: tt * 2048 + f4 * 512 + 512], ps[2 + f4][:, :], r2[:, tt:tt + 1], ALU.mult, [psk[2 + f4], "r2"], ["big"])
                for ft in range(16):
                    sv = s4[:, tt, ft, :]
                    fw.op("dve", lambda e, sv=sv, ft=ft: e.max(out=v16[:, ft, 0:8], in_=sv), ["big"], ["Dall"])
                    fw.op("dve", lambda e, sv=sv, ft=ft: e.match_replace(out=tmp128, in_to_replace=v16[:, ft, 0:8], in_values=sv, imm_value=-1e30), ["big", "Dall"], ["Dall"])
                    fw.op("dve", lambda e, ft=ft: e.max(out=v16[:, ft, 8:16], in_=tmp128), ["Dall"], ["Dall"])
                v4 = v16.rearrange("p (h two) a -> p h two a", two=2)
                k.tt("dve", cand.rearrange("p h (a b) -> p h a b", a=16),
                     v4[:, :, 0, :].unsqueeze(3).to_broadcast([128, 8, 16, 16]),
                     v4[:, :, 1, :].unsqueeze(2).to_broadcast([128, 8, 16, 16]), ALU.add, ["Dall"], ["Dall"])
                for h in range(8):
                    fw.op("dve", lambda e, h=h: e.max(out=top[:, h, 0:8], in_=cand[:, h, :]), ["Dall"], ["top"])
                    fw.op("dve", lambda e, h=h: e.match_replace(out=tmp256, in_to_replace=top[:, h, 0:8], in_values=cand[:, h, :], imm_value=-1e30), ["Dall", "top"], ["Dall"])
                    fw.op("dve", lambda e, h=h: e.max(out=top[:, h, 8:16], in_=tmp256), ["Dall"], ["top"])
                k.copy("dve", tau[:, tt, :], top[:, :, 15], ["top"], ["tau"])
                k.ts("dve", negm, top[:, :, 0], -1.0, ALU.mult, ["top"], ["negm"])
                for h in range(8):
                    k.act(junk16, top[:, h, :], AF.Exp, ["top", "negm"], ["junk16", "Zs"], bias=negm[:, h:h + 1], accum=Zs[:, h:h + 1])
                k.act(lnZ, Zs, AF.Ln, ["Zs"], ["lnZ"])
                k.tt("dve", nb[:, tt, :], negm, lnZ, ALU.subtract, ["negm", "lnZ"], ["nb"])
            units = [(ec, tt) for ec in range(32) for tt in range(4)]
            NU = len(units)

            def emit_dma(ec):
                rb = ec % 2
                fw.dma("pool", Rw[rb], I["ut"][ec].rearrange("p (k e) -> p k e", k=16), writes=["Rw%d" % rb], slot="Rw%d" % rb)
                fw.dma("pool", Rv[rb], I["vv"][ec].rearrange("p (j b) -> p j b", j=16), writes=["Rv%d" % rb], slot="Rv%d" % rb)

            def emit_A(u):
                ec, tt = units[u]
                rb = ec % 2
                pa = u % 2
                tk_ = slice(tt * 128, tt * 128 + 128)
                for kk in range(16):
                    k.mm(ps[pa][:, :], T1[:, kk, tk_], Rw[rb][:, kk, :], kk == 0, kk == 15, ["T1", "Rw%d" % rb], [psk[pa]], inc=(kk == 15))

            def emit_G(u):
                ec, tt = units[u]
                pa = u % 2
                k.act(gas[pa], ps[pa][:, :], AF.Gelu, [psk[pa], "r2"], ["ga%d" % pa], scale=r2[:, tt:tt + 1])

            def emit_Vmm(u):
                ec, tt = units[u]
                rb = ec % 2
                Vb = Rv[rb].rearrange("p (j a) b -> p j (a b)", a=4)
                wTu = wTs[u % 2]
                for n4 in range(4):
                    for et in range(4):
                        k.mm(ps[3 + n4][:, :], wTu[:, et, :], Vb[:, et, n4 * 512:(n4 + 1) * 512], et == 0, et == 3, ["wT%d" % (u % 2), "Rv%d" % rb], [psk[3 + n4]], inc=(et == 3))

            def emit_Vadd(u):
                ec, tt = units[u]
                for n4 in range(4):
                    k.tt("dve", res[:, tt, n4 * 512:(n4 + 1) * 512], ps[3 + n4][:, :], res[:, tt, n4 * 512:(n4 + 1) * 512], ALU.add, [psk[3 + n4], "res"], ["res"])

            emit_dma(0)
            emit_A(0)
            emit_G(0)
            for u in range(NU):
                ec, tt = units[u]
                pa = u % 2
                if u + 1 < NU:
                    if units[u + 1][1] == 0:
                        emit_dma(units[u + 1][0])
                    emit_A(u + 1)
                if u >= 1:
                    emit_Vmm(u - 1)
                s5 = s4[:, tt, :, :].rearrange("p (h two) n -> p h two n", two=2)
                for hh in range(2):
                    hs = slice(hh * 4, hh * 4 + 4)
                    k.tt("dve", Dall[:, hs, :].rearrange("p h (a b) -> p h a b", a=4),
                         s5[:, hs, 0, ec * 4:ec * 4 + 4].unsqueeze(3).to_broadcast([128, 4, 4, 128]),
                         s5[:, hs, 1, :].unsqueeze(2).to_broadcast([128, 4, 4, 128]), ALU.add, ["big", "Dall"], ["D%d" % hh, "Dall"])
                for h in range(8):
                    ek = "EX%d" % h
                    dk_ = "D%d" % (h // 4)
                    k.act(EX[:, h, :], Dall[:, h, :], AF.Exp, [dk_, "nb"], [ek], bias=nb[:, tt, h:h + 1])
                    k.stt(EX[:, h, :], Dall[:, h, :], tau[:, tt, h:h + 1], EX[:, h, :], ALU.is_ge, ALU.mult,
                          [dk_, ek, "tau"], [ek])
                if u + 1 < NU:
                    emit_G(u + 1)
                for h in range(8):
                    k.mm(ps[2][:, :], ident, EX[:, h, :], h == 0, h == 7, ["EX%d" % h, "cm"], ["ps2"], inc=(h == 7))
                if u >= 1:
                    emit_Vadd(u - 1)
                k.tt("dve", wb, ps[2][:, :], gas[pa], ALU.mult, ["ps2", "ga%d" % pa], ["wb"])
                p7 = psbf(7)
                for et in range(4):
                    k.tr(p7[:, et * 128:(et + 1) * 128], wb[:, et * 128:(et + 1) * 128], ident, ["wb", "cm"], ["ps7"])
                k.copy("act", wTs[pa].rearrange("p a b -> p (a b)"), p7[:, 0:512], ["ps7"], ["wT%d" % pa])
            emit_Vmm(NU - 1)
            emit_Vadd(NU - 1)
            fw.dma("sp", out_d[tsl, :].rearrange("(t p) d -> p t d", p=128), res, reads=["res"], writes=["out"], slot="out")
        fw.barrier(["sp"])
        fw.replay()
    return nc


def _host_inputs(inp):
    f = np.float32
    x = np.asarray(inp["x"], f)
    meta = np.asarray(inp["meta_tokens"], f)
    w_in = np.asarray(inp["w_in"], f)[0]
    common = {}
    def tile_cols(w, cw):
        K, N = w.shape
        return np.ascontiguousarray(w.reshape(K // 128, 128, N // cw, cw).transpose(2, 1, 0, 3)).reshape(N // cw, 128, (K // 128) * cw)

    common["w_gate"] = tile_cols(w_in[:, 4096:8192], 512)
    common["w_ab"] = tile_cols(np.asarray(inp["w_attn_branch"], f)[0], 512)
    common["w_glu"] = tile_cols(np.asarray(inp["w_glu"], f)[0], 512)
    common["w_out"] = tile_cols(np.asarray(inp["w_out"], f)[0], 512)
    common["w_q"] = tile_cols(np.asarray(inp["peer_w_q"], f)[0], 512)
    pu = np.asarray(inp["peer_u"], f)[0]
    common["ut"] = np.ascontiguousarray(pu.reshape(32, 512, 16, 128).transpose(0, 3, 2, 1)).reshape(32, 128, 16 * 512)
    pv = np.asarray(inp["peer_v"], f)[0]
    common["vv"] = np.ascontiguousarray(pv.reshape(32, 4, 128, 2048).transpose(0, 2, 1, 3)).reshape(32, 128, 4 * 2048)
    k1 = np.asarray(inp["peer_k1"], f)[0]
    k2 = np.asarray(inp["peer_k2"], f)[0]
    kt = np.stack([k1, k2], axis=1)
    common["kt"] = np.ascontiguousarray(kt.transpose(3, 0, 1, 2).reshape(128, 16 * 128))
    common["g1"] = np.ascontiguousarray(np.asarray(inp["norm1_g"], f)[0].reshape(16, 128).T)
    common["g2"] = np.ascontiguousarray(np.asarray(inp["norm2_g"], f)[0].reshape(16, 128).T)
    gq = np.tile(np.asarray(inp["q_norm_g"], f)[0], 2)
    gk = np.tile(np.asarray(inp["k_norm_g"], f)[0], 2)
    common["gqk"] = np.ascontiguousarray(np.stack([gq, gk], axis=1))
    lamv = np.concatenate([np.asarray(inp[n], f)[0] for n in ("lambda_q1", "lambda_k1", "lambda_q2", "lambda_k2")])
    common["lamv"] = np.ascontiguousarray(np.broadcast_to(lamv[None, :], (128, 256)))
    common["gsub"] = np.ascontiguousarray(np.broadcast_to(np.asarray(inp["subln_g"], f)[0][None, :], (128, 128)))
    wa = np.zeros((4, 2048, 1024), f)
    for p in range(4):
        blocks = []
        for off in (0, 1024, 2048, 3072):
            for l in range(2):
                t = 2 * p + l
                blocks.append(w_in[:, off + t * 128: off + t * 128 + 128])
        wa[p] = np.concatenate(blocks, axis=1)
    common["w_a"] = np.stack([tile_cols(wa[p], 1024)[0] for p in range(4)], axis=0)
    fidx = np.arange(128) % 64
    inv_freq = np.power(np.float32(10000.0), -(np.arange(32, dtype=f) / np.float32(32))).astype(f)
    sgn_r = np.where(fidx < 32, -1.0, 1.0).astype(f)
    ident = np.eye(128, dtype=f)
    partner = np.where(fidx < 32, np.arange(128) + 32, np.arange(128) - 32)
    perm = np.zeros((128, 128), f)
    perm[partner, np.arange(128)] = 1.0
    bones = (np.arange(128)[:, None] // 64 == np.arange(128)[None, :] // 64).astype(f)
    ones = np.ones((128, 128), f)
    common["cmat"] = np.ascontiguousarray(np.stack([ident, perm, bones, ones], axis=1).reshape(128, 512))
    masks = np.zeros((128, 4, 512), f)
    for i in range(4):
        masks[:, i, :] = (i * 128 + np.arange(128)[:, None] <= np.arange(512)[None, :]).astype(f)
    common["masks"] = masks.reshape(128, 4 * 512)
    rowmask = (np.arange(128)[:, None] // 16 == np.arange(8)[None, :]).astype(f)
    colmask = np.broadcast_to((np.arange(8)[:, None] == np.arange(128)[None, :] // 16).astype(f)[None], (128, 8, 128))
    sgn = np.concatenate([np.ones(64, f), -np.ones(64, f)])[:, None]
    common["smask"] = np.ascontiguousarray(np.concatenate([rowmask, colmask.reshape(128, 1024), sgn], axis=1))
    common["iota"] = np.ascontiguousarray(np.broadcast_to(np.arange(LP, dtype=f)[None, :], (128, LP)))

    a_re = np.asarray(inp["ssm_a_re"], f)[0]; a_im = np.asarray(inp["ssm_a_im"], f)[0]
    ldt = np.asarray(inp["ssm_log_dt"], f)[0]
    b_re = np.asarray(inp["ssm_b_re"], f)[0]; b_im = np.asarray(inp["ssm_b_im"], f)[0]
    c_re = np.asarray(inp["ssm_c_re"], f)[0]; c_im = np.asarray(inp["ssm_c_im"], f)[0]
    d_sk = np.asarray(inp["ssm_d"], f)[0]
    sa = np.zeros((8, 128, 128), f); sb = np.zeros((8, 128, 128), f); sldt = np.zeros((8, 128, 1), f)
    sS = np.zeros((8, 128, 24), f); sc = np.zeros((8, 128, 256), f); sd = np.zeros((8, 128, 1), f)
    for T in range(8):
        gs = slice(8 * T, 8 * T + 8)
        sa[T] = np.concatenate([np.repeat(a_re[gs], 16, axis=0), np.repeat(a_im[gs], 16, axis=0)], axis=1)
        sb[T] = np.concatenate([b_re[gs].transpose(0, 2, 1).reshape(128, 64), b_im[gs].transpose(0, 2, 1).reshape(128, 64)], axis=1)
        sldt[T] = np.repeat(ldt[gs], 16)[:, None]
        aT = np.concatenate([a_re[gs].T, a_re[gs].T], axis=0)
        iT = np.concatenate([a_im[gs].T, a_im[gs].T], axis=0)
        lT = np.broadcast_to(ldt[gs][None, :], (128, 8))
        sS[T] = np.concatenate([aT, iT, lT], axis=1)
        crT = c_re[gs].transpose(2, 0, 1).reshape(64, 128)
        ciT = c_im[gs].transpose(2, 0, 1).reshape(64, 128)
        sc[T] = np.concatenate([np.concatenate([crT, ciT], axis=0), np.concatenate([ciT, crT], axis=0)], axis=1)
        sd[T] = d_sk[T * 128:(T + 1) * 128][:, None]
    common.update(sa=sa, sb=sb, sldt=sldt, sS=sS, sc=sc, sd=sd)

    maps = []
    for c in range(NCORES):
        m = dict(common)
        beta, jq = c // 4, c % 4
        npad = 3072 - 1024 * jq + 112
        hT = np.zeros((2048, LP), f)
        hT[:, npad:npad + 16] = meta.T
        hT[:, npad + 16:] = x[beta, 0:1024 * (jq + 1), :].T
        m["hT"] = np.ascontiguousarray(hT.reshape(16, 128, 11, 384).transpose(2, 1, 0, 3)).reshape(11, 128, 16 * 384)
        xs = x[beta, jq * 1024:(jq + 1) * 1024, :]
        m["xrow"] = np.ascontiguousarray(xs)
        m["xTb"] = np.ascontiguousarray(xs.T)
        pos = np.maximum(np.arange(LP) - npad, 0).astype(f)
        ang = pos[None, :] * inv_freq[fidx % 32][:, None]
        m["ropec"] = np.cos(ang).astype(f)
        m["ropes"] = (np.sin(ang) * sgn_r[:, None]).astype(f)
        valid = (np.arange(LP) >= npad).astype(f)
        m["vvalid"] = np.ascontiguousarray(valid.reshape(33, 128).T)
        maps.append(m)
    return maps


def kernel(**inputs):
    maps = _host_inputs(inputs)
    nc = build_program()
    res = run_bass_kernel_spmd(nc, maps, core_ids=list(range(NCORES)))
    out = np.zeros((2, 4096, 2048), np.float32)
    for c in range(NCORES):
        out[c // 4, (c % 4) * 1024:(c % 4 + 1) * 1024, :] = res.results[c]["out"]
    return out
```

```python
import contextlib
import math
import numpy as np
import concourse.bass as bass
import concourse.mybir as mybir
from concourse.bass_utils import run_bass_kernel_spmd

F32 = mybir.dt.float32
BF16 = mybir.dt.bfloat16
I32 = mybir.dt.int32
ALU = mybir.AluOpType
AF = mybir.ActivationFunctionType
AX = mybir.AxisListType

ENGS = ["pe", "act", "dve", "pool", "sp"]
NCORES = 8
LP = 4224
NT = 2 * LP
EPS = 1e-6
LAM_INIT = 0.2
TWO_PI = 2.0 * math.pi
import os
DEBUG = bool(os.environ.get('KDEBUG'))


class FW:
    def __init__(self, nc, stack):
        self.nc = nc
        self.stack = stack
        self.ops = {e: [] for e in ENGS}
        self.ecnt = {e: 0 for e in ENGS}
        self.seen = {e: {} for e in ENGS}
        self.lastw = {}
        self.readers = {}
        self.dcnt = {}
        self.semobj = {}
        for e in ENGS:
            self.semobj["es_" + e] = stack.enter_context(nc.semaphore("es_" + e))

    def _slot(self, name):
        s = "ds_" + name
        if s not in self.semobj:
            self.semobj[s] = self.stack.enter_context(self.nc.semaphore(s))
            self.dcnt[s] = 0
        return s

    def _deps(self, reads, writes):
        ev = []
        for k in reads:
            if k in self.lastw:
                ev.append(self.lastw[k])
        for k in writes:
            if k in self.lastw:
                ev.append(self.lastw[k])
            ev.extend(self.readers.get(k, ()))
        return ev

    def _filter(self, e, evs):
        need = {}
        for (s, v) in evs:
            if s == "es_pe" and e == "pe":
                continue
            if v > need.get(s, 0):
                need[s] = v
        out = []
        for s, v in need.items():
            if self.seen[e].get(s, 0) >= v:
                continue
            self.seen[e][s] = v
            out.append((s, v))
        return out

    def _record(self, ev, reads, writes):
        for k in writes:
            self.lastw[k] = ev
            self.readers[k] = []
        for k in reads:
            self.readers.setdefault(k, []).append(ev)

    def op(self, e, fn, reads=(), writes=(), inc=True):
        waits = self._filter(e, self._deps(reads, writes))
        ev = ("es_" + e, self.ecnt[e] + 1)
        if inc:
            self.ecnt[e] += 1
        self.ops[e].append((waits, fn, ("es_" + e, 1) if inc else None))
        self._record(ev, reads, writes)

    def dma(self, q, out, in_, reads=(), writes=(), slot=None):
        s = self._slot(slot)
        waits = self._filter(q, self._deps(reads, writes))
        self.dcnt[s] += 16
        ev = (s, self.dcnt[s])
        self.ops[q].append((waits, lambda eng: eng.dma_start(out=out, in_=in_), (s, 16)))
        self._record(ev, reads, writes)

    def raw(self, e, fn, reads=(), writes=(), slot=None, incv=1):
        s = self._slot(slot)
        waits = self._filter(e, self._deps(reads, writes))
        self.dcnt[s] += incv
        ev = (s, self.dcnt[s])
        self.ops[e].append((waits, fn, (s, incv)))
        self._record(ev, reads, writes)

    def _all_events(self):
        evs = list(self.lastw.values())
        for l in self.readers.values():
            evs.extend(l)
        for en in ENGS:
            if self.ecnt[en] > 0:
                evs.append(("es_" + en, self.ecnt[en]))
        return evs

    def barrier(self, engines=ENGS):
        evs = self._all_events()
        for e in engines:
            waits = self._filter(e, evs)
            if waits:
                self.ops[e].append((waits, None, None))
        if len(engines) == len(ENGS):
            self.lastw = {}
            self.readers = {}

    def replay(self):
        fw = self

        def run(eng, lst):
            for waits, fn, inc in lst:
                for (s, v) in waits:
                    eng.wait_ge(fw.semobj[s], v)
                if fn is None:
                    continue
                ins = fn(eng)
                if inc is not None:
                    ins.then_inc(fw.semobj[inc[0]], inc[1])

        with self.nc.Block() as block:
            @block.tensor
            def _(eng):
                run(eng, fw.ops["pe"])

            @block.scalar
            def _(eng):
                run(eng, fw.ops["act"])

            @block.vector
            def _(eng):
                run(eng, fw.ops["dve"])

            @block.gpsimd
            def _(eng):
                run(eng, fw.ops["pool"])

            @block.sync
            def _(eng):
                run(eng, fw.ops["sp"])


def _reshape(ap, shape):
    if len(shape) == 2:
        return ap
    if len(shape) == 3:
        return ap.rearrange("p (a b) -> p a b", a=shape[1])
    if len(shape) == 4:
        return ap.rearrange("p (a b c) -> p a b c", a=shape[1], b=shape[2])
    raise ValueError(shape)


class Arena:
    def __init__(self, t, ncols):
        self.t = t
        self.n = ncols
        self.off = 0

    def reset(self):
        self.off = 0

    def _take(self, n32):
        c0 = self.off
        self.off += n32
        assert self.off <= self.n, ("arena overflow", self.off, self.n)
        return self.t[:, c0:c0 + n32]

    def f32(self, shape):
        n = int(np.prod(shape[1:]))
        return _reshape(self._take(n), shape)

    def i32(self, shape):
        n = int(np.prod(shape[1:]))
        return _reshape(self._take(n).bitcast(I32), shape)

    def bf16(self, shape):
        n = int(np.prod(shape[1:]))
        n32 = (n + 1) // 2
        return _reshape(self._take(n32).bitcast(BF16)[:, 0:n], shape)


class K:
    def __init__(self, fw):
        self.fw = fw

    def mm(self, out, lhsT, rhs, start, stop, r, w, inc=True):
        self.fw.op("pe", lambda e: e.matmul(out, lhsT=lhsT, rhs=rhs, start=start, stop=stop), r, w, inc)

    def tr(self, out, in_, ident, r, w, inc=True):
        self.fw.op("pe", lambda e: e.transpose(out, in_, ident), r, w, inc)

    def act(self, out, in_, func, r, w, scale=None, bias=None, accum=None):
        kw = {}
        if scale is not None:
            kw["scale"] = scale
        if bias is not None:
            kw["bias"] = bias
        if accum is not None:
            kw["accum_out"] = accum
        self.fw.op("act", lambda e: e.activation(out=out, in_=in_, func=func, **kw), r, w)

    def tt(self, eng, out, in0, in1, op, r, w):
        self.fw.op(eng, lambda e: e.tensor_tensor(out=out, in0=in0, in1=in1, op=op), r, w)

    def ts(self, eng, out, in0, s1, op0, r, w, s2=None, op1=None):
        if s2 is None:
            self.fw.op(eng, lambda e: e.tensor_scalar(out=out, in0=in0, scalar1=s1, scalar2=None, op0=op0), r, w)
        else:
            self.fw.op(eng, lambda e: e.tensor_scalar(out=out, in0=in0, scalar1=s1, scalar2=s2, op0=op0, op1=op1), r, w)

    def stt(self, out, in0, scalar, in1, op0, op1, r, w):
        self.fw.op("dve", lambda e: e.scalar_tensor_tensor(out=out, in0=in0, scalar=scalar, in1=in1, op0=op0, op1=op1), r, w)

    def copy(self, eng, out, in_, r, w):
        if eng == "act":
            self.fw.op("act", lambda e: e.activation(out=out, in_=in_, func=AF.Copy), r, w)
        else:
            self.fw.op(eng, lambda e: e.tensor_copy(out=out, in_=in_), r, w)

    def recip(self, out, in_, r, w):
        self.fw.op("dve", lambda e: e.reciprocal(out=out, in_=in_), r, w)

    def memset(self, eng, ap, val, w):
        self.fw.op(eng, lambda e: e.memset(ap, val), (), w)

    def rsqrt(self, out, in_, scale, epsc, r, w):
        self.act(out, in_, AF.Sqrt, r, w, scale=scale, bias=epsc)
        self.recip(out, out, w, w)


def build_program():
    nc = bass.Bass("TRN2", target_bir_lowering=False)
    I = {}

    def inp(name, shape):
        I[name] = nc.dram_tensor(name, list(shape), F32, kind="ExternalInput").ap()

    inp("hT", [11, 128, 16 * 384])
    inp("vvalid", [128, 33])
    inp("xrow", [1024, 2048])
    inp("xTb", [2048, 1024])
    inp("w_a", [4, 128, 16 * 1024])
    inp("w_gate", [8, 128, 16 * 512])
    inp("w_ab", [4, 128, 8 * 512])
    inp("w_glu", [8, 128, 8 * 512])
    inp("w_out", [4, 128, 16 * 512])
    inp("w_q", [4, 128, 16 * 512])
    inp("ut", [32, 128, 16 * 512])
    inp("vv", [32, 128, 4 * 2048])
    inp("kt", [128, 16 * 128])
    inp("g1", [128, 16])
    inp("g2", [128, 16])
    inp("gqk", [128, 2])
    inp("lamv", [128, 4 * 64])
    inp("gsub", [128, 128])
    inp("ropec", [128, LP])
    inp("ropes", [128, LP])
    inp("cmat", [128, 4 * 128])
    inp("masks", [128, 4 * 512])
    inp("sa", [8, 128, 2 * 64])
    inp("sb", [8, 128, 2 * 64])
    inp("sldt", [8, 128, 1])
    inp("sS", [8, 128, 3 * 8])
    inp("sc", [8, 128, 2 * 128])
    inp("sd", [8, 128, 1])
    inp("smask", [128, 8 + 8 * 128 + 1])
    inp("iota", [128, LP])
    out_d = nc.dram_tensor("out", [1024, 2048], F32, kind="ExternalOutput").ap()
    gsc = nc.dram_tensor("gsc", [2048, 512], F32).ap()
    gsc_b = gsc.bitcast(BF16)

    with contextlib.ExitStack() as st:
        fw = FW(nc, st)
        k = K(fw)
        ARENA_COLS = 53200
        arena_t = st.enter_context(nc.sbuf_tensor("arena", [128, ARENA_COLS], F32))
        ar = Arena(arena_t, ARENA_COLS)
        _pa = [st.enter_context(nc.psum_tensor("bank%d" % i, [128, 512], F32)) for i in range(3)]
        psbig = st.enter_context(nc.psum_tensor("bank3456", [128, 2048], F32))
        _pb = st.enter_context(nc.psum_tensor("bank7", [128, 512], F32))
        ps = _pa + [psbig[:, i * 512:(i + 1) * 512] for i in range(4)] + [_pb]
        psk = ["ps%d" % i for i in range(8)]

        def psbf(i):
            return ps[i][:, :].bitcast(BF16)

        cm = ar.bf16([128, 4, 128])
        ident, perm, bones, ones = cm[:, 0, :], cm[:, 1, :], cm[:, 2, :], cm[:, 3, :]
        fw.dma("pool", cm, I["cmat"].rearrange("p (a b) -> p a b", a=4), writes=["cm"], slot="s_cm")
        msk = ar.bf16([128, 4, 512])
        fw.dma("pool", msk, I["masks"].rearrange("p (a b) -> p a b", a=4), writes=["msk"], slot="s_msk")
        g1 = ar.f32([128, 16]); fw.dma("sp", g1, I["g1"], writes=["g1"], slot="s_g1")
        gqk = ar.f32([128, 2]); fw.dma("sp", gqk, I["gqk"], writes=["gqk"], slot="s_gqk")
        lamv = ar.f32([128, 4, 64])
        fw.dma("sp", lamv, I["lamv"].rearrange("p (a b) -> p a b", a=4), writes=["lamv"], slot="s_lamv")
        gsub = ar.f32([128, 128]); fw.dma("sp", gsub, I["gsub"], writes=["gsub"], slot="s_gsub")
        vval = ar.f32([128, 33]); fw.dma("sp", vval, I["vvalid"], writes=["vval"], slot="s_vval")
        smk = ar.f32([128, 8 + 1024 + 1]); fw.dma("sp", smk, I["smask"], writes=["smk"], slot="s_smk")
        rowmask = smk[:, 0:8]
        colmask = smk[:, 8:8 + 1024].rearrange("p (a b) -> p a b", a=8)
        sgn = smk[:, 1032:1033]
        iota = ar.f32([128, LP]); fw.dma("sp", iota, I["iota"], writes=["iota"], slot="s_iota")
        epsc = ar.f32([128, 1])
        k.memset("dve", epsc, EPS, ["epsc"])
        onec = ar.f32([128, 1])
        k.memset("dve", onec, 1.0, ["onec"])
        magc = ar.f32([128, 2])
        k.memset("dve", magc[:, 0:1], 12582912.0, ["magc"])
        k.memset("dve", magc[:, 1:2], -12582912.0, ["magc"])
        k.ts("dve", gsub, gsub, 1.0 - LAM_INIT, ALU.mult, ["gsub"], ["gsub"])
        lp = ar.f32([128, 2, 64]); lsum = ar.f32([128, 2]); lexp = ar.f32([128, 2]); neglam = ar.f32([128, 1])
        k.tt("dve", lp[:, 0, :], lamv[:, 0, :], lamv[:, 1, :], ALU.mult, ["lamv"], ["lp"])
        k.tt("dve", lp[:, 1, :], lamv[:, 2, :], lamv[:, 3, :], ALU.mult, ["lamv"], ["lp"])
        fw.op("dve", lambda e: e.tensor_reduce(out=lsum, in_=lp, axis=AX.X, op=ALU.add), ["lp"], ["lsum"])
        k.act(lexp, lsum, AF.Exp, ["lsum"], ["lexp"])
        k.tt("dve", neglam, lexp[:, 1:2], lexp[:, 0:1], ALU.subtract, ["lexp"], ["neglam"])
        k.ts("dve", neglam, neglam, -LAM_INIT, ALU.add, ["neglam"], ["neglam"])
        rstd_all = ar.f32([128, LP])
        mark_pass = ar.off
        hT = I["hT"]
        WIN0 = LP - 1024
        MAGIC = 12582912.0

        for p in range(4):
            fw.barrier()
            ar.off = mark_pass
            Wa = ar.bf16([128, 16, 1024])
            fw.dma("pool", Wa, I["w_a"][p].rearrange("p (k f) -> p k f", k=16), writes=["Wa"], slot="s_wa")
            for kk in range(16):
                k.ts("dve", Wa[:, kk, :], Wa[:, kk, :], g1[:, kk:kk + 1], ALU.mult, ["Wa", "g1"], ["Wa"])
            QT = ar.bf16([128, 2, 1152])
            KT = ar.bf16([128, 2, LP])
            UT = ar.bf16([128, 2, LP])
            Vtok = ar.bf16([128, 33, 2, 130])
            for hl in range(2):
                k.copy("dve", Vtok[:, :, hl, 128:129], vval.unsqueeze(2), ["vval"], ["Vtok"])
            mark_a = ar.off
            xbs = [ar.bf16([128, 16, 384]) for _ in range(2)]
            xsq = ar.bf16([128, 16, 384])
            rcs = [ar.f32([128, 384]) for _ in range(2)]
            rss = [ar.f32([128, 384]) for _ in range(2)]
            qs_ = [ar.f32([128, 384]) for _ in range(2)]; sqb_ = [ar.bf16([128, 384]) for _ in range(2)]
            rq_ = [ar.f32([128, 384]) for _ in range(2)]; qn_ = [ar.bf16([128, 384]) for _ in range(2)]
            t1_ = [ar.f32([128, 384]) for _ in range(2)]; t2_ = [ar.f32([128, 384]) for _ in range(2)]
            vsb_ = [ar.bf16([128, 384]) for _ in range(2)]
            fc = 0
            pbc = 0
            for ci in range(11):
                b = ci % 2
                col0 = ci * 384
                cols = slice(col0, col0 + 384)
                xb = xbs[b]
                xbk = "xb%d" % b
                fw.dma("pool", xb, hT[ci].rearrange("p (k t) -> p k t", k=16), writes=[xbk], slot=xbk)
                fw.dma("sp", rcs[b], I["ropec"][:, cols], writes=["rc%d" % b], slot="rc%d" % b)
                fw.dma("sp", rss[b], I["ropes"][:, cols], writes=["rs%d" % b], slot="rs%d" % b)
                rstd = rstd_all[:, cols]
                if p == 0:
                    k.act(xsq, xb, AF.Square, [xbk], ["xsq"])
                    for kk in range(16):
                        k.mm(ps[0][:, 0:384], ones, xsq[:, kk, :], kk == 0, kk == 15, ["cm", "xsq"], ["ps0"], inc=(kk == 15))
                    k.rsqrt(rstd, ps[0][:, 0:384], 1.0 / 2048, epsc, ["ps0", "epsc"], ["rstd"])
                fts = [("k", 0, 2), ("k", 1, 3), ("v", 0, 4), ("v", 1, 5), ("u", 0, 6), ("u", 1, 7)]
                if ci >= 8:
                    fts = [("q", 0, 0), ("q", 1, 1)] + fts
                for kind, hl, f in fts:
                    pb = 1 + pbc % 4
                    pbc += 1
                    pbk = psk[pb]
                    for kk in range(16):
                        k.mm(ps[pb][:, 0:384], Wa[:, kk, f * 128:(f + 1) * 128], xb[:, kk, :], kk == 0, kk == 15,
                             ["Wa", xbk], [pbk], inc=(kk == 15))
                    if kind in ("q", "k"):
                        gi = 0 if kind == "q" else 1
                        z = fc % 2
                        fc += 1
                        qs, sqb, rq, qn, t1, t2 = qs_[z], sqb_[z], rq_[z], qn_[z], t1_[z], t2_[z]
                        zs = str(z)
                        pn = 5 + z
                        k.tt("dve", qs, ps[pb][:, 0:384], rstd, ALU.mult, [pbk, "rstd"], ["qs" + zs])
                        k.act(sqb, qs, AF.Square, ["qs" + zs], ["sqb" + zs])
                        k.mm(ps[pn][:, 0:384], bones, sqb, True, True, ["cm", "sqb" + zs], [psk[pn]])
                        k.rsqrt(rq, ps[pn][:, 0:384], 1.0 / 64, epsc, [psk[pn], "epsc"], ["rq" + zs])
                        k.stt(qn, qs, gqk[:, gi:gi + 1], rq, ALU.mult, ALU.mult, ["qs" + zs, "gqk", "rq" + zs], ["qn" + zs])
                        k.mm(ps[pn][:, 0:384], perm, qn, True, True, ["cm", "qn" + zs], [psk[pn]])
                        k.tt("dve", t1, qn, rcs[b], ALU.mult, ["qn" + zs, "rc%d" % b], ["t1" + zs])
                        k.tt("dve", t2, ps[pn][:, 0:384], rss[b], ALU.mult, [psk[pn], "rs%d" % b], ["t2" + zs])
                        if kind == "q":
                            dst = QT[:, hl, (ci - 8) * 384:(ci - 8) * 384 + 384]
                            dk = "QT"
                        else:
                            dst = KT[:, hl, cols]
                            dk = "KT"
                        k.tt("dve", dst, t1, t2, ALU.add, ["t1" + zs, "t2" + zs], [dk])
                    elif kind == "v":
                        z = fc % 2
                        fc += 1
                        vsb = vsb_[z]
                        k.tt("dve", vsb, ps[pb][:, 0:384], rstd, ALU.mult, [pbk, "rstd"], ["vsb%d" % z])
                        p7 = psbf(7)
                        for j in range(3):
                            k.tr(p7[:, j * 128:(j + 1) * 128], vsb[:, j * 128:(j + 1) * 128], ident, ["vsb%d" % z, "cm"], ["ps7"])
                        k.copy("act", Vtok[:, ci * 3:ci * 3 + 3, hl, 0:128],
                               p7[:, 0:384].rearrange("p (a b) -> p a b", a=3), ["ps7"], ["Vtok"])
                    else:
                        k.tt("dve", UT[:, hl, cols], ps[pb][:, 0:384], rstd, ALU.mult, [pbk, "rstd"], ["UT"])

            fw.barrier()
            ar.off = mark_a
            Pb = [ar.bf16([128, 512]) for _ in range(2)]
            Asb = [ar.f32([128, 4, 128]) for _ in range(2)]
            rl = ar.f32([128, 1])
            attf = ar.f32([128, 4, 128])
            junk = ar.f32([128, 128])
            ss4 = ar.f32([128, 4])
            attn = ar.bf16([128, 4, 128])
            attT = [ar.bf16([128, 512]) for _ in range(2)]
            ai = 0
            sa = ar.f32([128, 2, 64]); sbb = ar.f32([128, 2, 64]); sldt = ar.f32([128, 1])
            sS = ar.f32([128, 3, 8]); scc = ar.f32([128, 2, 128]); sd_ = [ar.f32([128, 1]) for _ in range(2)]
            dtc = ar.f32([128, 1])
            ard = ar.f32([128, 64]); aid = ar.f32([128, 64]); rr = ar.f32([128, 64])
            yy = ar.f32([128, 64]); kf = ar.f32([128, 64]); ff = ar.f32([128, 64])
            sn = ar.f32([128, 64]); hh = ar.f32([128, 64]); cs = ar.f32([128, 64])
            abr = ar.f32([128, 64]); abi = ar.f32([128, 64]); den = ar.f32([128, 64]); tq = ar.f32([128, 64])
            mr = ar.f32([128, 64]); mi = ar.f32([128, 64]); Bre = ar.f32([128, 64]); Bim = ar.f32([128, 64])
            Bpad_ = [ar.bf16([128, 8, 128]) for _ in range(2)]; Bswp_ = [ar.bf16([128, 8, 128]) for _ in range(2)]
            CAs = ar.f32([128, 128]); CBs = ar.f32([128, 128])
            CpA_ = [ar.bf16([128, 8, 128]) for _ in range(2)]; CpB_ = [ar.bf16([128, 8, 128]) for _ in range(2)]
            dtS = ar.f32([128, 8]); rS_ = [ar.f32([128, 8]) for _ in range(2)]; thS_ = [ar.f32([128, 8]) for _ in range(2)]
            carr = ar.f32([128, 8])
            TC = 512
            tys = [ar.f32([128, TC]) for _ in range(2)]; tkf = ar.f32([128, TC])
            COS = [ar.f32([128, TC]) for _ in range(2)]
            SIN = [ar.f32([128, TC]) for _ in range(2)]
            th_ = ar.f32([128, TC])
            w1s = [ar.f32([128, TC]) for _ in range(2)]; w2s = [ar.f32([128, TC]) for _ in range(2)]; zzs = [ar.f32([128, TC]) for _ in range(2)]
            u1 = [ar.bf16([128, TC]) for _ in range(2)]
            u2 = [ar.bf16([128, TC]) for _ in range(2)]
            ysf = ar.f32([128, TC])
            ysb = [ar.bf16([128, TC]) for _ in range(2)]
            W = ["ssmtmp"]
            ti = 0
            yic = [0]

            def ssm_setup(tl):
                Bpad, Bswp, CpA, CpB, rS, thS, sd = Bpad_[tl], Bswp_[tl], CpA_[tl], CpB_[tl], rS_[tl], thS_[tl], sd_[tl]
                kB, kC, kr, kth, ksd = 'Bpad%d' % tl, 'Cpad%d' % tl, 'rS%d' % tl, 'thS%d' % tl, 'sd%d' % tl
                T = 2 * p + tl
                fw.dma("sp", sa, I["sa"][T].rearrange("p (a b) -> p a b", a=2), writes=["sa"], slot="s_sa")
                fw.dma("sp", sbb, I["sb"][T].rearrange("p (a b) -> p a b", a=2), writes=["sbb"], slot="s_sb")
                fw.dma("sp", sldt, I["sldt"][T], writes=["sldt"], slot="s_sldt")
                fw.dma("sp", sS, I["sS"][T].rearrange("p (a b) -> p a b", a=3), writes=["sS"], slot="s_sS")
                fw.dma("sp", scc, I["sc"][T].rearrange("p (a b) -> p a b", a=2), writes=["scc"], slot="s_sc")
                fw.dma("sp", sd, I["sd"][T], writes=[ksd], slot="s_sd")
                k.act(dtc, sldt, AF.Exp, ["sldt"], W)
                k.ts("dve", ard, sa[:, 0, :], dtc[:, 0:1], ALU.mult, ["sa"] + W, W)
                k.ts("dve", aid, sa[:, 1, :], dtc[:, 0:1], ALU.mult, ["sa"] + W, W)
                k.act(rr, ard, AF.Exp, W, W)
                k.ts("dve", yy, aid, 1.0 / TWO_PI, ALU.mult, W, W)
                k.ts("dve", kf, yy, MAGIC, ALU.add, W, W)
                k.ts("dve", kf, kf, -MAGIC, ALU.add, W, W)
                k.tt("dve", ff, yy, kf, ALU.subtract, W, W)
                k.act(sn, ff, AF.Sin, W, W, scale=TWO_PI)
                k.act(hh, ff, AF.Sin, W, W, scale=math.pi)
                k.tt("dve", hh, hh, hh, ALU.mult, W, W)
                k.ts("dve", cs, hh, -2.0, ALU.mult, W, W, s2=1.0, op1=ALU.add)
                k.tt("dve", abr, rr, cs, ALU.mult, W, W)
                k.ts("dve", abr, abr, -1.0, ALU.add, W, W)
                k.tt("dve", abi, rr, sn, ALU.mult, W, W)
                k.tt("dve", den, sa[:, 0, :], sa[:, 0, :], ALU.mult, ["sa"] + W, W)
                k.tt("dve", tq, sa[:, 1, :], sa[:, 1, :], ALU.mult, ["sa"] + W, W)
                k.tt("dve", den, den, tq, ALU.add, W, W)
                k.recip(den, den, W, W)
                k.tt("dve", mr, abr, sa[:, 0, :], ALU.mult, ["sa"] + W, W)
                k.tt("dve", tq, abi, sa[:, 1, :], ALU.mult, ["sa"] + W, W)
                k.tt("dve", mr, mr, tq, ALU.add, W, W)
                k.tt("dve", mr, mr, den, ALU.mult, W, W)
                k.tt("dve", mi, abi, sa[:, 0, :], ALU.mult, ["sa"] + W, W)
                k.tt("dve", tq, abr, sa[:, 1, :], ALU.mult, ["sa"] + W, W)
                k.tt("dve", mi, mi, tq, ALU.subtract, W, W)
                k.tt("dve", mi, mi, den, ALU.mult, W, W)
                k.tt("dve", Bre, mr, sbb[:, 0, :], ALU.mult, ["sbb"] + W, W)
                k.tt("dve", tq, mi, sbb[:, 1, :], ALU.mult, ["sbb"] + W, W)
                k.tt("dve", Bre, Bre, tq, ALU.subtract, W, W)
                k.tt("dve", Bim, mr, sbb[:, 1, :], ALU.mult, ["sbb"] + W, W)
                k.tt("dve", tq, mi, sbb[:, 0, :], ALU.mult, ["sbb"] + W, W)
                k.tt("dve", Bim, Bim, tq, ALU.add, W, W)
                rmb = rowmask.unsqueeze(2).to_broadcast([128, 8, 64])
                breb = Bre.unsqueeze(1).to_broadcast([128, 8, 64])
                bimb = Bim.unsqueeze(1).to_broadcast([128, 8, 64])
                k.tt("dve", Bpad[:, :, 0:64], breb, rmb, ALU.mult, ["smk"] + W, [kB])
                k.tt("dve", Bpad[:, :, 64:128], bimb, rmb, ALU.mult, ["smk"] + W, [kB])
                k.tt("dve", Bswp[:, :, 0:64], bimb, rmb, ALU.mult, ["smk"] + W, [kB])
                k.ts("dve", tq, Bre, -1.0, ALU.mult, W, W)
                k.tt("dve", Bswp[:, :, 64:128], tq.unsqueeze(1).to_broadcast([128, 8, 64]), rmb, ALU.mult, ["smk"] + W, [kB])
                k.ts("dve", CAs, scc[:, 0, :], sgn, ALU.mult, ["scc", "smk"], W)
                k.ts("dve", CBs, scc[:, 1, :], -1.0, ALU.mult, ["scc"], W)
                k.tt("dve", CpA, CAs.unsqueeze(1).to_broadcast([128, 8, 128]), colmask, ALU.mult, ["smk"] + W, [kC])
                k.tt("dve", CpB, CBs.unsqueeze(1).to_broadcast([128, 8, 128]), colmask, ALU.mult, ["smk"] + W, [kC])
                k.act(dtS, sS[:, 2, :], AF.Exp, ["sS"], ["dtS"])
                k.tt("dve", rS, sS[:, 0, :], dtS, ALU.mult, ["sS", "dtS"], [kr])
                k.act(rS, rS, AF.Exp, [kr], [kr])
                k.tt("dve", thS, sS[:, 1, :], dtS, ALU.mult, ["sS", "dtS"], [kth])
                k.ts("dve", thS, thS, 1.0 / TWO_PI, ALU.mult, [kth], [kth])

            def ssm_loop(tl):
                Bpad, Bswp, CpA, CpB, rS, thS, sd = Bpad_[tl], Bswp_[tl], CpA_[tl], CpB_[tl], rS_[tl], thS_[tl], sd_[tl]
                kB, kC, kr, kth, ksd = 'Bpad%d' % tl, 'Cpad%d' % tl, 'rS%d' % tl, 'thS%d' % tl, 'sd%d' % tl
                T = 2 * p + tl
                sunits = [(tc, g) for tc in range(9) for g in range(8)]

                def dims(tc):
                    n = TC if tc < 8 else LP - 8 * TC
                    return n, tc * TC

                def emit_tab1a(ui):
                    tc, g = sunits[ui]
                    n, p0_ = dims(tc)
                    cols = slice(p0_, p0_ + n)
                    k.act(tkf[:, 0:n], iota[:, cols], AF.Identity, ["iota", kth, "magc"], ["tkf"], scale=thS[:, g:g + 1], bias=magc[:, 0:1])
                    k.act(tkf[:, 0:n], tkf[:, 0:n], AF.Identity, ["tkf", "magc"], ["tkf"], bias=magc[:, 1:2])

                def emit_tab1b(ui):
                    tc, g = sunits[ui]
                    n, p0_ = dims(tc)
                    cols = slice(p0_, p0_ + n)
                    tyb = tys[ui % 2]
                    k.stt(tyb[:, 0:n], iota[:, cols], thS[:, g:g + 1], tkf[:, 0:n], ALU.mult, ALU.subtract, ["iota", kth, "tkf"], ["ty%d" % (ui % 2)])

                def emit_tab2(ui):
                    tc, g = sunits[ui]
                    n, p0_ = dims(tc)
                    tb = ui % 2
                    tk = "tab%d" % tb
                    tyb = tys[tb]
                    k.act(SIN[tb][:, 0:n], tyb[:, 0:n], AF.Sin, ["ty%d" % tb], [tk], scale=TWO_PI)
                    k.act(th_[:, 0:n], tyb[:, 0:n], AF.Sin, ["ty%d" % tb], ["th_"], scale=math.pi)
                    k.act(th_[:, 0:n], th_[:, 0:n], AF.Square, ["th_"], ["th_"])
                    k.act(COS[tb][:, 0:n], th_[:, 0:n], AF.Identity, ["th_", "onec"], [tk], scale=-2.0, bias=onec)

                NS = len(sunits)
                emit_tab1a(0); emit_tab1b(0); emit_tab2(0)
                emit_tab1a(1); emit_tab1b(1)
                for ui in range(NS):
                    tc, g = sunits[ui]
                    n, p0_ = dims(tc)
                    cols = slice(p0_, p0_ + n)
                    outp = tc >= 6
                    tb = ui % 2
                    tk = "tab%d" % tb
                    w1, w2, zz = w1s[tb], w2s[tb], zzs[tb]
                    wk1, wk2, zk = "w1%d" % tb, "w2%d" % tb, "zz%d" % tb
                    b1, b2 = (0, 1) if tb == 0 else (3, 4)
                    k.mm(ps[b1][:, 0:n], Bpad[:, g, :], UT[:, tl, cols], True, True, [kB, "UT"], [psk[b1]])
                    k.mm(ps[b2][:, 0:n], Bswp[:, g, :], UT[:, tl, cols], True, True, [kB, "UT"], [psk[b2]])
                    if ui + 1 < NS:
                        emit_tab2(ui + 1)
                    if ui + 2 < NS:
                        emit_tab1a(ui + 2)
                    k.tt("dve", w1[:, 0:n], ps[b1][:, 0:n], COS[tb][:, 0:n], ALU.mult, [psk[b1], tk], [wk1])
                    k.tt("dve", w2[:, 0:n], ps[b2][:, 0:n], SIN[tb][:, 0:n], ALU.mult, [psk[b2], tk], [wk2])
                    k.tt("dve", w1[:, 0:n], w1[:, 0:n], w2[:, 0:n], ALU.add, [wk1, wk2], [wk1])
                    rsb = rS[:, g:g + 1].to_broadcast([128, n])
                    init = 0.0 if tc == 0 else carr[:, g:g + 1]

                    def scan(e, rsb=rsb, init=init, n=n, zz=zz, w1=w1):
                        return e.tensor_tensor_scan(out=zz[:, 0:n], data0=rsb, data1=w1[:, 0:n], initial=init,
                                                    op0=ALU.mult, op1=ALU.add)
                    fw.op("dve", scan, [kr, wk1, "carr"], [zk])
                    k.copy("dve", carr[:, g:g + 1], zz[:, n - 1:n], [zk], ["carr"])
                    if ui + 2 < NS:
                        emit_tab1b(ui + 2)
                    if outp:
                        ub = g % 2
                        k.tt("dve", u1[ub][:, 0:n], zz[:, 0:n], COS[tb][:, 0:n], ALU.mult, [zk, tk], ["u1%d" % ub])
                        k.tt("dve", u2[ub][:, 0:n], zz[:, 0:n], SIN[tb][:, 0:n], ALU.mult, [zk, tk], ["u2%d" % ub])
                        k.mm(ps[2][:, 0:n], CpA[:, g, :], u1[ub][:, 0:n], g == 0, False, [kC, "u1%d" % ub], ["ps2"])
                        k.mm(ps[2][:, 0:n], CpB[:, g, :], u2[ub][:, 0:n], False, g == 7, [kC, "u2%d" % ub], ["ps2"])
                    if outp and g == 7:
                        k.stt(ysf[:, 0:n], UT[:, tl, cols], sd[:, 0:1], ps[2][:, 0:n], ALU.mult, ALU.add,
                              ["UT", ksd, "ps2"], ["ysf"])
                        yb = ysb[yic[0] % 2]
                        yk = "ysb%d" % (yic[0] % 2)
                        yic[0] += 1
                        k.act(yb[:, 0:n], ysf[:, 0:n], AF.Gelu, ["ysf"], [yk])
                        lo = max(p0_, WIN0)
                        r0 = T * 256 + 128
                        fw.dma("sp", gsc_b[r0:r0 + 128, lo - WIN0:p0_ + n - WIN0], yb[:, lo - p0_:n],
                               reads=[yk], writes=["gsc"], slot="gsc")

            ssm_setup(0)
            ssm_setup(1)
            for hl in range(2):
                for qc in range(2):
                    kd = 25 + 4 * qc
                    nkt = kd + 4
                    for sub in range(2):
                        pr = slice(sub * 64, sub * 64 + 64)
                        def emit_s(kt_):
                            sb_ = kt_ % 2
                            k.mm(ps[sb_][:, :], KT[pr, hl, kt_ * 128:kt_ * 128 + 128],
                                 QT[pr, hl, 128 + qc * 512:128 + qc * 512 + 512], True, True, ["KT", "QT"], [psk[sb_]])
                            k.act(Pb[sb_], ps[sb_][:, :], AF.Exp, [psk[sb_]], ["P%d" % sb_], scale=0.125)

                        emit_s(0)
                        for kt_ in range(nkt):
                            sb_ = kt_ % 2
                            if kt_ + 1 < nkt:
                                emit_s(kt_ + 1)
                            P = Pb[sb_]
                            pk = "P%d" % sb_
                            i = kt_ - kd
                            if i >= 0:
                                k.tt("dve", P, P, msk[:, i, :], ALU.mult, [pk, "msk"], [pk])
                            for j in range(4):
                                if kt_ <= kd + j:
                                    k.mm(ps[2 + j][:, 0:129], P[:, j * 128:(j + 1) * 128], Vtok[:, kt_, hl, 0:129],
                                         kt_ == 0, kt_ == kd + j, [pk, "Vtok"], [psk[2 + j]])
                        for j in range(4):
                            k.recip(rl, ps[2 + j][:, 128:129], [psk[2 + j]], ["rl"])
                            k.ts("dve", Asb[sub][:, j, :], ps[2 + j][:, 0:128], rl[:, 0:1], ALU.mult,
                                 [psk[2 + j], "rl"], ["A%d" % sub])
                    for j in range(4):
                        k.stt(attf[:, j, :], Asb[1][:, j, :], neglam[:, 0:1], Asb[0][:, j, :], ALU.mult, ALU.add,
                              ["A0", "A1", "neglam"], ["attf"])
                        k.act(junk, attf[:, j, :], AF.Square, ["attf"], ["junk", "ss4"], accum=ss4[:, j:j + 1])
                    k.rsqrt(ss4, ss4, 1.0 / 128, epsc, ["ss4", "epsc"], ["ss4"])
                    p6 = psbf(6)
                    for j in range(4):
                        k.stt(attn[:, j, :], attf[:, j, :], ss4[:, j:j + 1], gsub, ALU.mult, ALU.mult,
                              ["attf", "ss4", "gsub"], ["attn"])
                        k.tr(p6[:, j * 128:(j + 1) * 128], attn[:, j, :], ident, ["attn", "cm"], ["ps6"])
                    aT = attT[ai % 2]
                    ak = "attT%d" % (ai % 2)
                    ai += 1
                    k.copy("act", aT, p6[:, 0:512], ["ps6"], [ak])
                    r0 = (2 * p + hl) * 256
                    fw.dma("sp", gsc_b[r0:r0 + 128, qc * 512:qc * 512 + 512], aT, reads=[ak], writes=["gsc"], slot="gsc")

            ssm_loop(0)
            ssm_loop(1)
        if DEBUG:
            dbg = nc.dram_tensor("dbg_gsc", [2048, 512], F32, kind="ExternalOutput").ap()
            fw.dma("sp", dbg, gsc, reads=["gsc"], writes=["dbg"], slot="dbg")
        fw.barrier()

        ar.reset()
        cm = ar.bf16([128, 4, 128])
        ident = cm[:, 0, :]
        ar.off = mark_pb = ar.off
        zeroT = cm[:, 1, :]
        k.memset("dve", zeroT, 0.0, ["cm"])
        g1 = ar.f32([128, 16]); fw.dma("sp", g1, I["g1"], writes=["g1"], slot="s_g1")
        g2 = ar.f32([128, 16]); fw.dma("sp", g2, I["g2"], writes=["g2"], slot="s_g2")
        epsc = ar.f32([128, 1]); k.memset("dve", epsc, EPS, ["epsc"])
        KTb = ar.bf16([128, 16, 128])
        fw.dma("pool", KTb, I["kt"].rearrange("p (a b) -> p a b", a=16), writes=["KTb"], slot="s_kt")
        res = ar.f32([128, 4, 2048])
        T1 = ar.bf16([128, 16, 512])
        T2 = ar.bf16([128, 16, 512])
        big = ar._take(8192)
        mix = _reshape(big[:, 0:4096].bitcast(BF16), [128, 4, 2048])
        sfl = big
        s4 = big.rearrange("p (t f n) -> p t f n", t=4, f=16)
        Rw = [ar.bf16([128, 16, 512]) for _ in range(2)]
        Rv = [ar.bf16([128, 16, 512]) for _ in range(2)]
        scr = ar.bf16([128, 2048])
        ssq = ar.f32([128, 4]); r1 = ar.f32([128, 4]); r2 = ar.f32([128, 4])
        sab = ar.bf16([128, 512]); sbb2 = ar.bf16([128, 512]); sgb = ar.bf16([128, 512])
        ta = ar.f32([128, 512]); tb2 = ar.f32([128, 512])
        Dall = ar.f32([128, 8, 512])
        Dflat = Dall.rearrange("p h e -> p (h e)")
        cand = Dflat[:, 0:2048].rearrange("p (h e) -> p h e", h=8)
        tmp256 = Dflat[:, 2048:2304]
        tmp128 = Dflat[:, 2304:2432]
        v16 = Dflat[:, 2432:2688].rearrange("p (a b) -> p a b", a=16)
        top = ar.f32([128, 8, 16])
        tau = ar.f32([128, 4, 8]); nb = ar.f32([128, 4, 8])
        negm = ar.f32([128, 8]); Zs = ar.f32([128, 8]); lnZ = ar.f32([128, 8])
        junk16 = ar.f32([128, 16])
        gas = [ar.bf16([128, 512]) for _ in range(2)]
        wb = ar.bf16([128, 512])
        EX = ar.bf16([128, 8, 512])
        wTs = [ar.bf16([128, 4, 128]) for _ in range(2)]


        for hf in range(2):
            tsl = slice(hf * 512, hf * 512 + 512)
            fw.dma("sp", res, I["xrow"][tsl, :].rearrange("(t p) d -> p t d", p=128), writes=["res"], slot="res")
            fw.dma("pool", T1, I["xTb"][:, tsl].rearrange("(k p) t -> p k t", p=128), writes=["T1"], slot="T1")
            gv = gsc_b[:, tsl].rearrange("(r a p) t -> a p r t", a=2, p=128)
            for a in range(2):
                fw.dma("sp", T2[:, a * 8:a * 8 + 8, :], gv[a], reads=["gsc"], writes=["T2"], slot="T2")
            for tt in range(4):
                k.act(scr, res[:, tt, :], AF.Square, ["res"], ["scr", "ssq"], accum=ssq[:, tt:tt + 1])
            k.rsqrt(r1, ssq, 1.0 / 2048, epsc, ["ssq", "epsc"], ["r1"])
            k.tt("dve", T1, T1, g1.unsqueeze(2).to_broadcast([128, 16, 512]), ALU.mult, ["T1", "g1"], ["T1"])
            for n4 in range(4):
                c0 = n4 * 512
                fw.dma("pool", Rw[0], I["w_gate"][n4].rearrange("p (k f) -> p k f", k=16), writes=["Rw0"], slot="Rw0")
                fw.dma("pool", Rw[1], I["w_gate"][4 + n4].rearrange("p (k f) -> p k f", k=16), writes=["Rw1"], slot="Rw1")
                fw.dma("pool", Rv[0][:, 0:8, :], I["w_ab"][n4].rearrange("p (k f) -> p k f", k=8), writes=["Rv0"], slot="Rv0")
                fw.dma("pool", Rv[1][:, 0:8, :], I["w_glu"][n4].rearrange("p (k f) -> p k f", k=8), writes=["Rv1"], slot="Rv1")
                fw.dma("pool", Rv[1][:, 8:16, :], I["w_glu"][4 + n4].rearrange("p (k f) -> p k f", k=8), writes=["Rv1"], slot="Rv1")
                for tt in range(4):
                    tk_ = slice(tt * 128, tt * 128 + 128)
                    for kk in range(16):
                        k.mm(ps[0][:, :], T1[:, kk, tk_], Rw[0][:, kk, :], kk == 0, kk == 15, ["T1", "Rw0"], ["ps0"], inc=(kk == 15))
                    for kk in range(16):
                        k.mm(ps[1][:, :], T1[:, kk, tk_], Rw[1][:, kk, :], kk == 0, kk == 15, ["T1", "Rw1"], ["ps1"], inc=(kk == 15))
                    for kk in range(8):
                        k.mm(ps[2][:, :], T2[:, kk, tk_], Rv[0][:, kk, :], kk == 0, kk == 7, ["T2", "Rv0"], ["ps2"], inc=(kk == 7))
                    for kk in range(8):
                        k.mm(ps[3][:, :], T2[:, 8 + kk, tk_], Rv[1][:, kk, :], kk == 0, kk == 7, ["T2", "Rv1"], ["ps3"], inc=(kk == 7))
                    for kk in range(8):
                        k.mm(ps[4][:, :], T2[:, 8 + kk, tk_], Rv[1][:, 8 + kk, :], kk == 0, kk == 7, ["T2", "Rv1"], ["ps4"], inc=(kk == 7))
                    k.act(sab, ps[0][:, :], AF.Sigmoid, ["ps0", "r1"], ["sab"], scale=r1[:, tt:tt + 1])
                    k.act(sbb2, ps[1][:, :], AF.Sigmoid, ["ps1", "r1"], ["sbb2"], scale=r1[:, tt:tt + 1])
                    k.act(sgb, ps[4][:, :], AF.Sigmoid, ["ps4"], ["sgb"])
                    k.tt("dve", ta, ps[2][:, :], sab, ALU.mult, ["ps2", "sab"], ["ta"])
                    k.tt("dve", tb2, ps[3][:, :], sgb, ALU.mult, ["ps3", "sgb"], ["tb2"])
                    k.tt("pool", tb2, tb2, sbb2, ALU.mult, ["tb2", "sbb2"], ["tb2"])
                    k.tt("pool", mix[:, tt, c0:c0 + 512], ta, tb2, ALU.add, ["ta", "tb2"], ["big"])
            p7 = psbf(7)
            for tt in range(4):
                for half8 in range(2):
                    for kk in range(8):
                        kf_ = half8 * 8 + kk
                        k.tr(p7[:, kk * 128:(kk + 1) * 128], mix[:, tt, kf_ * 128:(kf_ + 1) * 128], ident, ["big", "cm"], ["ps7"])
                    k.copy("act", T2[:, half8 * 8:half8 * 8 + 8, tt * 128:(tt + 1) * 128],
                           p7.rearrange("p (a b) -> p a b", a=8), ["ps7"], ["T2"])
            for n4 in range(4):
                c0 = n4 * 512
                rb = n4 % 2
                fw.dma("pool", Rw[rb], I["w_out"][n4].rearrange("p (k f) -> p k f", k=16), writes=["Rw%d" % rb], slot="Rw%d" % rb)
                for tt in range(4):
                    tk_ = slice(tt * 128, tt * 128 + 128)
                    pb_ = tt % 2
                    for kk in range(16):
                        k.mm(ps[pb_][:, :], T2[:, kk, tk_], Rw[rb][:, kk, :], kk == 0, kk == 15, ["T2", "Rw%d" % rb], [psk[pb_]], inc=(kk == 15))
                    k.tt("dve", res[:, tt, c0:c0 + 512], ps[pb_][:, :], res[:, tt, c0:c0 + 512], ALU.add, [psk[pb_], "res"], ["res"])
            for tt in range(4):
                k.act(scr, res[:, tt, :], AF.Square, ["res"], ["scr", "ssq"], accum=ssq[:, tt:tt + 1])
            k.rsqrt(r2, ssq, 1.0 / 2048, epsc, ["ssq", "epsc"], ["r2"])
            for tt in range(4):
                k.copy("act", scr, res[:, tt, :], ["res"], ["scr"])
                for half8 in range(2):
                    for kk in range(8):
                        kf_ = half8 * 8 + kk
                        k.tr(p7[:, kk * 128:(kk + 1) * 128], scr[:, kf_ * 128:(kf_ + 1) * 128], ident, ["scr", "cm"], ["ps7"])
                    k.copy("dve", T1[:, half8 * 8:half8 * 8 + 8, tt * 128:(tt + 1) * 128],
                           p7.rearrange("p (a b) -> p a b", a=8), ["ps7"], ["T1"])
            k.tt("dve", T1, T1, g2.unsqueeze(2).to_broadcast([128, 16, 512]), ALU.mult, ["T1", "g2"], ["T1"])
            for wc in range(4):
                rb = wc % 2
                fw.dma("pool", Rw[rb], I["w_q"][wc].rearrange("p (k f) -> p k f", k=16), writes=["Rw%d" % rb], slot="Rw%d" % rb)
                for f4 in range(4):
                    ft = wc * 4 + f4
                    pb_ = f4 % 2
                    for kk in range(16):
                        k.mm(ps[pb_][:, :], Rw[rb][:, kk, f4 * 128:(f4 + 1) * 128], T1[:, kk, :], kk == 0, kk == 15, ["T1", "Rw%d" % rb], [psk[pb_]], inc=(kk == 15))
                    k.copy("act", T2[:, ft, :], ps[pb_][:, :], [psk[pb_]], ["T2"])
            for tt in range(4):
                tk_ = slice(tt * 128, tt * 128 + 128)
                for ft in range(16):
                    pb_ = 2 + ft // 4
                    k.mm(ps[pb_][:, (ft % 4) * 128:(ft % 4) * 128 + 128], T2[:, ft, tk_], KTb[:, ft, :], True, True, ["T2", "KTb"], [psk[pb_]])
                for f4 in range(4):
                    k.ts("dve", sfl[:, tt * 2048 + f4 * 512: tt * 2048 + f4 * 512 + 512], ps[2 + f4][:, :], r2[:, tt:tt + 1], ALU.mult, [psk[2 + f4], "r2"], ["big"])
                for ft in range(16):
                    sv = s4[:, tt, ft, :]
                    fw.op("dve", lambda e, sv=sv, ft=ft: e.max(out=v16[:, ft, 0:8], in_=sv), ["big"], ["Dall"])
                    fw.op("dve", lambda e, sv=sv, ft=ft: e.match_replace(out=tmp128, in_to_replace=v16[:, ft, 0:8], in_values=sv, imm_value=-1e30), ["big", "Dall"], ["Dall"])
                    fw.op("dve", lambda e, ft=ft: e.max(out=v16[:, ft, 8:16], in_=tmp128), ["Dall"], ["Dall"])
                v4 = v16.rearrange("p (h two) a -> p h two a", two=2)
                k.tt("dve", cand.rearrange("p h (a b) -> p h a b", a=16),
                     v4[:, :, 0, :].unsqueeze(3).to_broadcast([128, 8, 16, 16]),
                     v4[:, :, 1, :].unsqueeze(2).to_broadcast([128, 8, 16, 16]), ALU.add, ["Dall"], ["Dall"])
                for h in range(8):
                    fw.op("dve", lambda e, h=h: e.max(out=top[:, h, 0:8], in_=cand[:, h, :]), ["Dall"], ["top"])
                    fw.op("dve", lambda e, h=h: e.match_replace(out=tmp256, in_to_replace=top[:, h, 0:8], in_values=cand[:, h, :], imm_value=-1e30), ["Dall", "top"], ["Dall"])
                    fw.op("dve", lambda e, h=h: e.max(out=top[:, h, 8:16], in_=tmp256), ["Dall"], ["top"])
                k.copy("dve", tau[:, tt, :], top[:, :, 15], ["top"], ["tau"])
                k.ts("dve", negm, top[:, :, 0], -1.0, ALU.mult, ["top"], ["negm"])
                for h in range(8):
                    k.act(junk16, top[:, h, :], AF.Exp, ["top", "negm"], ["junk16", "Zs"], bias=negm[:, h:h + 1], accum=Zs[:, h:h + 1])
                k.act(lnZ, Zs, AF.Ln, ["Zs"], ["lnZ"])
                k.tt("dve", nb[:, tt, :], negm, lnZ, ALU.subtract, ["negm", "lnZ"], ["nb"])
            units = [(ec, tt) for ec in range(32) for tt in range(4)]
            NU = len(units)

            def emit_dma(ec):
                rb = ec % 2
                fw.dma("pool", Rw[rb], I["ut"][ec].rearrange("p (k e) -> p k e", k=16), writes=["Rw%d" % rb], slot="Rw%d" % rb)
                fw.dma("pool", Rv[rb], I["vv"][ec].rearrange("p (j b) -> p j b", j=16), writes=["Rv%d" % rb], slot="Rv%d" % rb)

            def emit_A(u):
                ec, tt = units[u]
                rb = ec % 2
                pa = u % 2
                tk_ = slice(tt * 128, tt * 128 + 128)
                for kk in range(16):
                    k.mm(ps[pa][:, :], T1[:, kk, tk_], Rw[rb][:, kk, :], kk == 0, kk == 15, ["T1", "Rw%d" % rb], [psk[pa]], inc=(kk == 15))

            def emit_G(u):
                ec, tt = units[u]
                pa = u % 2
                k.act(gas[pa], ps[pa][:, :], AF.Gelu, [psk[pa], "r2"], ["ga%d" % pa], scale=r2[:, tt:tt + 1])

            def emit_Vmm(u):
                ec, tt = units[u]
                rb = ec % 2
                Vb = Rv[rb].rearrange("p (j a) b -> p j (a b)", a=4)
                wTu = wTs[u % 2]
                for n4 in range(4):
                    for et in range(4):
                        k.mm(ps[3 + n4][:, :], wTu[:, et, :], Vb[:, et, n4 * 512:(n4 + 1) * 512], et == 0, et == 3, ["wT%d" % (u % 2), "Rv%d" % rb], [psk[3 + n4]], inc=(et == 3))

            def emit_Vadd(u):
                ec, tt = units[u]
                k.tt("dve", res[:, tt, :], psbig[:, :], res[:, tt, :], ALU.add, [psk[3], psk[4], psk[5], psk[6], "res"], ["res"])

            emit_dma(0)
            emit_A(0)
            emit_G(0)
            for u in range(NU):
                ec, tt = units[u]
                pa = u % 2
                if u + 1 < NU:
                    if units[u + 1][1] == 0:
                        emit_dma(units[u + 1][0])
                    emit_A(u + 1)
                if u >= 1:
                    emit_Vmm(u - 1)
                s5 = s4[:, tt, :, :].rearrange("p (h two) n -> p h two n", two=2)
                for hh in range(2):
                    hs = slice(hh * 4, hh * 4 + 4)
                    k.tt("dve", Dall[:, hs, :].rearrange("p h (a b) -> p h a b", a=4),
                         s5[:, hs, 0, ec * 4:ec * 4 + 4].unsqueeze(3).to_broadcast([128, 4, 4, 128]),
                         s5[:, hs, 1, :].unsqueeze(2).to_broadcast([128, 4, 4, 128]), ALU.add, ["big", "Dall"], ["D%d" % hh, "Dall"])
                for h in range(8):
                    ek = "EX%d" % h
                    dk_ = "D%d" % (h // 4)
                    k.act(EX[:, h, :], Dall[:, h, :], AF.Exp, [dk_, "nb"], [ek], bias=nb[:, tt, h:h + 1])
                    k.stt(EX[:, h, :], Dall[:, h, :], tau[:, tt, h:h + 1], EX[:, h, :], ALU.is_ge, ALU.mult,
                          [dk_, ek, "tau"], [ek])
                if u + 1 < NU:
                    emit_G(u + 1)
                for h in range(8):
                    k.mm(ps[2][:, :], ident, EX[:, h, :], h == 0, h == 7, ["EX%d" % h, "cm"], ["ps2"], inc=(h == 7))
                if u >= 1:
                    emit_Vadd(u - 1)
                k.tt("dve", wb, ps[2][:, :], gas[pa], ALU.mult, ["ps2", "ga%d" % pa], ["wb"])
                p7 = psbf(7)
                for et in range(4):
                    k.tr(p7[:, et * 128:(et + 1) * 128], wb[:, et * 128:(et + 1) * 128], ident, ["wb", "cm"], ["ps7"])
                k.copy("act", wTs[pa].rearrange("p a b -> p (a b)"), p7[:, 0:512], ["ps7"], ["wT%d" % pa])
            emit_Vmm(NU - 1)
            emit_Vadd(NU - 1)
            fw.dma("sp", out_d[tsl, :].rearrange("(t p) d -> p t d", p=128), res, reads=["res"], writes=["out"], slot="out")
        fw.barrier(["sp"])
        fw.replay()
    return nc


def _host_inputs(inp):
    f = np.float32
    x = np.asarray(inp["x"], f)
    meta = np.asarray(inp["meta_tokens"], f)
    w_in = np.asarray(inp["w_in"], f)[0]
    common = {}
    def tile_cols(w, cw):
        K, N = w.shape
        return np.ascontiguousarray(w.reshape(K // 128, 128, N // cw, cw).transpose(2, 1, 0, 3)).reshape(N // cw, 128, (K // 128) * cw)

    common["w_gate"] = tile_cols(w_in[:, 4096:8192], 512)
    common["w_ab"] = tile_cols(np.asarray(inp["w_attn_branch"], f)[0], 512)
    common["w_glu"] = tile_cols(np.asarray(inp["w_glu"], f)[0], 512)
    common["w_out"] = tile_cols(np.asarray(inp["w_out"], f)[0], 512)
    common["w_q"] = tile_cols(np.asarray(inp["peer_w_q"], f)[0], 512)
    pu = np.asarray(inp["peer_u"], f)[0]
    common["ut"] = np.ascontiguousarray(pu.reshape(32, 512, 16, 128).transpose(0, 3, 2, 1)).reshape(32, 128, 16 * 512)
    pv = np.asarray(inp["peer_v"], f)[0]
    common["vv"] = np.ascontiguousarray(pv.reshape(32, 4, 128, 2048).transpose(0, 2, 1, 3)).reshape(32, 128, 4 * 2048)
    k1 = np.asarray(inp["peer_k1"], f)[0]
    k2 = np.asarray(inp["peer_k2"], f)[0]
    kt = np.stack([k1, k2], axis=1)
    common["kt"] = np.ascontiguousarray(kt.transpose(3, 0, 1, 2).reshape(128, 16 * 128))
    common["g1"] = np.ascontiguousarray(np.asarray(inp["norm1_g"], f)[0].reshape(16, 128).T)
    common["g2"] = np.ascontiguousarray(np.asarray(inp["norm2_g"], f)[0].reshape(16, 128).T)
    gq = np.tile(np.asarray(inp["q_norm_g"], f)[0], 2)
    gk = np.tile(np.asarray(inp["k_norm_g"], f)[0], 2)
    common["gqk"] = np.ascontiguousarray(np.stack([gq, gk], axis=1))
    lamv = np.concatenate([np.asarray(inp[n], f)[0] for n in ("lambda_q1", "lambda_k1", "lambda_q2", "lambda_k2")])
    common["lamv"] = np.ascontiguousarray(np.broadcast_to(lamv[None, :], (128, 256)))
    common["gsub"] = np.ascontiguousarray(np.broadcast_to(np.asarray(inp["subln_g"], f)[0][None, :], (128, 128)))
    wa = np.zeros((4, 2048, 1024), f)
    for p in range(4):
        blocks = []
        for off in (0, 1024, 2048, 3072):
            for l in range(2):
                t = 2 * p + l
                blocks.append(w_in[:, off + t * 128: off + t * 128 + 128])
        wa[p] = np.concatenate(blocks, axis=1)
    common["w_a"] = np.stack([tile_cols(wa[p], 1024)[0] for p in range(4)], axis=0)
    fidx = np.arange(128) % 64
    inv_freq = np.power(np.float32(10000.0), -(np.arange(32, dtype=f) / np.float32(32))).astype(f)
    sgn_r = np.where(fidx < 32, -1.0, 1.0).astype(f)
    ident = np.eye(128, dtype=f)
    partner = np.where(fidx < 32, np.arange(128) + 32, np.arange(128) - 32)
    perm = np.zeros((128, 128), f)
    perm[partner, np.arange(128)] = 1.0
    bones = (np.arange(128)[:, None] // 64 == np.arange(128)[None, :] // 64).astype(f)
    ones = np.ones((128, 128), f)
    common["cmat"] = np.ascontiguousarray(np.stack([ident, perm, bones, ones], axis=1).reshape(128, 512))
    masks = np.zeros((128, 4, 512), f)
    for i in range(4):
        masks[:, i, :] = (i * 128 + np.arange(128)[:, None] <= np.arange(512)[None, :]).astype(f)
    common["masks"] = masks.reshape(128, 4 * 512)
    rowmask = (np.arange(128)[:, None] // 16 == np.arange(8)[None, :]).astype(f)
    colmask = np.broadcast_to((np.arange(8)[:, None] == np.arange(128)[None, :] // 16).astype(f)[None], (128, 8, 128))
    sgn = np.concatenate([np.ones(64, f), -np.ones(64, f)])[:, None]
    common["smask"] = np.ascontiguousarray(np.concatenate([rowmask, colmask.reshape(128, 1024), sgn], axis=1))
    common["iota"] = np.ascontiguousarray(np.broadcast_to(np.arange(LP, dtype=f)[None, :], (128, LP)))

    a_re = np.asarray(inp["ssm_a_re"], f)[0]; a_im = np.asarray(inp["ssm_a_im"], f)[0]
    ldt = np.asarray(inp["ssm_log_dt"], f)[0]
    b_re = np.asarray(inp["ssm_b_re"], f)[0]; b_im = np.asarray(inp["ssm_b_im"], f)[0]
    c_re = np.asarray(inp["ssm_c_re"], f)[0]; c_im = np.asarray(inp["ssm_c_im"], f)[0]
    d_sk = np.asarray(inp["ssm_d"], f)[0]
    sa = np.zeros((8, 128, 128), f); sb = np.zeros((8, 128, 128), f); sldt = np.zeros((8, 128, 1), f)
    sS = np.zeros((8, 128, 24), f); sc = np.zeros((8, 128, 256), f); sd = np.zeros((8, 128, 1), f)
    for T in range(8):
        gs = slice(8 * T, 8 * T + 8)
        sa[T] = np.concatenate([np.repeat(a_re[gs], 16, axis=0), np.repeat(a_im[gs], 16, axis=0)], axis=1)
        sb[T] = np.concatenate([b_re[gs].transpose(0, 2, 1).reshape(128, 64), b_im[gs].transpose(0, 2, 1).reshape(128, 64)], axis=1)
        sldt[T] = np.repeat(ldt[gs], 16)[:, None]
        aT = np.concatenate([a_re[gs].T, a_re[gs].T], axis=0)
        iT = np.concatenate([a_im[gs].T, a_im[gs].T], axis=0)
        lT = np.broadcast_to(ldt[gs][None, :], (128, 8))
        sS[T] = np.concatenate([aT, iT, lT], axis=1)
        crT = c_re[gs].transpose(2, 0, 1).reshape(64, 128)
        ciT = c_im[gs].transpose(2, 0, 1).reshape(64, 128)
        sc[T] = np.concatenate([np.concatenate([crT, ciT], axis=0), np.concatenate([ciT, crT], axis=0)], axis=1)
        sd[T] = d_sk[T * 128:(T + 1) * 128][:, None]
    common.update(sa=sa, sb=sb, sldt=sldt, sS=sS, sc=sc, sd=sd)

    maps = []
    for c in range(NCORES):
        m = dict(common)
        beta, jq = c // 4, c % 4
        npad = 3072 - 1024 * jq + 112
        hT = np.zeros((2048, LP), f)
        hT[:, npad:npad + 16] = meta.T
        hT[:, npad + 16:] = x[beta, 0:1024 * (jq + 1), :].T
        m["hT"] = np.ascontiguousarray(hT.reshape(16, 128, 11, 384).transpose(2, 1, 0, 3)).reshape(11, 128, 16 * 384)
        xs = x[beta, jq * 1024:(jq + 1) * 1024, :]
        m["xrow"] = np.ascontiguousarray(xs)
        m["xTb"] = np.ascontiguousarray(xs.T)
        pos = np.maximum(np.arange(LP) - npad, 0).astype(f)
        ang = pos[None, :] * inv_freq[fidx % 32][:, None]
        m["ropec"] = np.cos(ang).astype(f)
        m["ropes"] = (np.sin(ang) * sgn_r[:, None]).astype(f)
        valid = (np.arange(LP) >= npad).astype(f)
        m["vvalid"] = np.ascontiguousarray(valid.reshape(33, 128).T)
        maps.append(m)
    return maps


def kernel(**inputs):
    maps = _host_inputs(inputs)
    nc = build_program()
    res = run_bass_kernel_spmd(nc, maps, core_ids=list(range(NCORES)))
    out = np.zeros((2, 4096, 2048), np.float32)
    for c in range(NCORES):
        out[c // 4, (c % 4) * 1024:(c % 4 + 1) * 1024, :] = res.results[c]["out"]
    return out
```
